# Optimizing a Trainium2 kernel written in Bass

```python
import numpy as np
import jax
import jax.numpy as jnp
from jax import lax


D_MODEL = 4096
BATCH = 4
SEQ = 4096
DEPTH = 2

GRID_W = 64
CTX_LEN = 256
EPS = 1e-6
NEG_INF = -1e30
ROPE_BASE = 10000.0
ATTN_QBLOCK = 128

RET_HEADS = 8
RET_DK = 256
RET_DV = 256
RET_CHUNK = 128
NA_HEADS = 16
NA_HD = 128
NA_KH = 8
NA_KW = 16
NA_QB = 16
NA_KB = NA_QB + NA_KW
MLA_HEADS = 16
MLA_Q_RANK = 1536
MLA_KV_RANK = 512
MLA_NOPE = 128
MLA_ROPE = 64
MLA_V = 128
LRU_WIDTH = 2048
LRU_BLOCKS = 16
LRU_BD = LRU_WIDTH // LRU_BLOCKS
LRU_CONV = 4
LRU_C = 8.0
FFN_DENSE = 11008
N_EXPERTS = 8
TOP_K = 2
FFN_EXPERT = 4096

N_EVEN = (DEPTH + 1) // 2
N_ODD = DEPTH // 2
EVEN_SPLITS = (RET_HEADS * RET_DK, RET_HEADS * RET_DK, RET_HEADS * RET_DV, RET_HEADS * RET_DV, NA_HEADS * NA_HD, NA_HEADS * NA_HD, NA_HEADS * NA_HD)
EVEN_IN = sum(EVEN_SPLITS)
EVEN_OUT = RET_HEADS * RET_DV + NA_HEADS * NA_HD
ODD_SPLITS = (MLA_Q_RANK, MLA_KV_RANK, MLA_ROPE, LRU_WIDTH, LRU_WIDTH)
ODD_IN = sum(ODD_SPLITS)
ODD_OUT = MLA_HEADS * MLA_V + LRU_WIDTH

kernel_name = 'hybrid_retention_na_mla_rglru_dit_block'


def _rms_norm(x, g):
    xf = x.astype(jnp.float32)
    y = xf * lax.rsqrt(jnp.mean(xf * xf, axis=-1, keepdims=True) + EPS)
    return (y * g.astype(jnp.float32)).astype(x.dtype)


def _split(t, sizes):
    return jnp.split(t, [int(v) for v in np.cumsum(sizes)[:-1]], axis=-1)


def _heads(t, n):
    b, l, _ = t.shape
    return t.reshape(b, l, n, -1).transpose(0, 2, 1, 3)


def _merge(t):
    b, h, l, d = t.shape
    return t.transpose(0, 2, 1, 3).reshape(b, l, h * d)


def _axial_rope(n, rot_dim):
    t = jnp.arange(n)
    row = (t // GRID_W).astype(jnp.float32)
    col = (t % GRID_W).astype(jnp.float32)
    nf = rot_dim // 4
    inv = ROPE_BASE ** (-jnp.arange(nf, dtype=jnp.float32) / nf)
    ar = row[:, None] * inv
    ac = col[:, None] * inv
    return (jnp.cos(ar), jnp.sin(ar), jnp.cos(ac), jnp.sin(ac))


def _apply_rope(x, tab):
    cr, sr, cc, sc = tab
    x1, x2, x3, x4 = jnp.split(x.astype(jnp.float32), 4, axis=-1)
    out = jnp.concatenate([x1 * cr - x2 * sr, x2 * cr + x1 * sr, x3 * cc - x4 * sc, x4 * cc + x3 * sc], axis=-1)
    return out.astype(x.dtype)


def _blocked_attention(q, k, v, scale):
    b, h, n, dq = q.shape
    nb = n // ATTN_QBLOCK
    qb = jnp.moveaxis(q.reshape(b, h, nb, ATTN_QBLOCK, dq), 2, 0)

    def one_block(qblk):
        s = jnp.einsum('bhqd,bhkd->bhqk', qblk, k).astype(jnp.float32) * scale
        p = jax.nn.softmax(s, axis=-1).astype(v.dtype)
        return jnp.einsum('bhqk,bhkd->bhqd', p, v)

    o = lax.map(one_block, qb)
    return jnp.moveaxis(o, 0, 2).reshape(b, h, n, v.shape[-1])


def _retention_scan(q, k, v, log_gamma, state0):
    b, h, n, _ = q.shape
    nc = n // RET_CHUNK
    pos = jnp.arange(RET_CHUNK, dtype=jnp.float32)
    lg = log_gamma[:, None]
    diff = pos[:, None] - pos[None, :]
    intra = jnp.where(diff >= 0, jnp.exp(log_gamma[:, None, None] * jnp.maximum(diff, 0.0)), 0.0)
    q_dec = jnp.exp(lg * (pos + 1.0))[None, :, :, None]
    k_dec = jnp.exp(lg * (RET_CHUNK - 1.0 - pos))[None, :, :, None]
    s_dec = jnp.exp(log_gamma * RET_CHUNK)[None, :, None, None]

    def chunks(t):
        return jnp.moveaxis(t.astype(jnp.float32).reshape(b, h, nc, RET_CHUNK, t.shape[-1]), 2, 0)

    def step(state, qkv):
        qc, kc, vc = qkv
        att = jnp.einsum('bhid,bhjd->bhij', qc, kc) * intra
        out = jnp.einsum('bhij,bhjv->bhiv', att, vc) + jnp.einsum('bhid,bhdv->bhiv', qc * q_dec, state)
        state = state * s_dec + jnp.einsum('bhjd,bhjv->bhdv', kc * k_dec, vc)
        return state, out

    state, out = lax.scan(step, state0, (chunks(q), chunks(k), chunks(v)))
    return jnp.moveaxis(out, 0, 2).reshape(b, h, n, -1), state


def _retention_bidir(q_c, k_c, v_c, q_l, k_l, v_l, log_gamma):
    b, h = q_c.shape[:2]
    zero = jnp.zeros((b, h, RET_DK, RET_DV), jnp.float32)
    fl = lambda t: jnp.flip(t, axis=2)
    oc_f, sc_f = _retention_scan(q_c, k_c, v_c, log_gamma[0], zero)
    ol_f, _ = _retention_scan(q_l, k_l, v_l, log_gamma[0], sc_f)
    oc_b, sc_b = _retention_scan(fl(q_c), fl(k_c), fl(v_c), log_gamma[1], zero)
    ol_b, _ = _retention_scan(fl(q_l), fl(k_l), fl(v_l), log_gamma[1], sc_b)
    return oc_f + fl(oc_b), ol_f + fl(ol_b)


def _head_rms(y):
    return y * lax.rsqrt(jnp.mean(y * y, axis=-1, keepdims=True) + EPS)


def _na_latent(q, k, v, k_ctx, v_ctx, rpb):
    b, h, s, d = q.shape
    rows = s // GRID_W
    kh = min(NA_KH, rows)
    nqb = GRID_W // NA_QB
    qcol = np.arange(GRID_W).reshape(nqb, NA_QB)
    kstart = np.clip(qcol[:, 0] - NA_KW // 2, 0, GRID_W - NA_KB)
    kcol = kstart[:, None] + np.arange(NA_KB)
    cstart = np.clip(qcol - NA_KW // 2, 0, GRID_W - NA_KW)
    col_ok = (kcol[:, None, :] >= cstart[:, :, None]) & (kcol[:, None, :] < cstart[:, :, None] + NA_KW)
    dc_idx = np.clip(kcol[:, None, :] - qcol[:, :, None] + NA_KW - 1, 0, 2 * NA_KW - 2)
    rpb_cols = rpb[:, :, dc_idx]
    mask = col_ok[:, :, None, :]
    scale = d ** -0.5
    kg = k.reshape(b, h, rows, GRID_W, d)
    vg = v.reshape(b, h, rows, GRID_W, d)
    qg = jnp.moveaxis(q.reshape(b, h, rows, nqb, NA_QB, d), 2, 0)
    n_loc = kh * NA_KB

    def one_row(args):
        q_row, r = args
        r0 = jnp.clip(r - kh // 2, 0, rows - kh)
        k_blk = lax.dynamic_slice_in_dim(kg, r0, kh, axis=2)[:, :, :, kcol]
        v_blk = lax.dynamic_slice_in_dim(vg, r0, kh, axis=2)[:, :, :, kcol]
        s_loc = jnp.einsum('bhnqd,bhrnkd->bhnqrk', q_row, k_blk).astype(jnp.float32) * scale
        bias = jnp.take(rpb_cols, r0 + jnp.arange(kh) - r + NA_KH - 1, axis=1).astype(jnp.float32)
        s_loc = jnp.where(mask, s_loc + jnp.transpose(bias, (0, 2, 3, 1, 4)), NEG_INF)
        s_ctx = jnp.einsum('bhnqd,bhld->bhnql', q_row, k_ctx).astype(jnp.float32) * scale
        p = jax.nn.softmax(jnp.concatenate([s_loc.reshape(b, h, nqb, NA_QB, n_loc), s_ctx], axis=-1), axis=-1).astype(v.dtype)
        p_loc = p[..., :n_loc].reshape(b, h, nqb, NA_QB, kh, NA_KB)
        return jnp.einsum('bhnqrk,bhrnkd->bhnqd', p_loc, v_blk) + jnp.einsum('bhnql,bhld->bhnqd', p[..., n_loc:], v_ctx)

    out = lax.map(one_row, (qg, jnp.arange(rows)))
    return jnp.moveaxis(out, 0, 2).reshape(b, h, s, d)


def _even_mixer(hc, hl, w_in, w_out, ret_decay, rpb, rope_ret, need_ctx):
    log_g = jax.nn.log_sigmoid(ret_decay.astype(jnp.float32))
    rq_c, rk_c, rv_c, rg_c, nq_c, nk_c, nv_c = _split(hc @ w_in, EVEN_SPLITS)
    rq_l, rk_l, rv_l, rg_l, nq_l, nk_l, nv_l = _split(hl @ w_in, EVEN_SPLITS)
    kscale = RET_DK ** -0.5
    q_c, k_c, v_c = _heads(rq_c, RET_HEADS), _heads(rk_c, RET_HEADS) * kscale, _heads(rv_c, RET_HEADS)
    q_l = _apply_rope(_heads(rq_l, RET_HEADS), rope_ret)
    k_l = _apply_rope(_heads(rk_l, RET_HEADS), rope_ret) * kscale
    v_l = _heads(rv_l, RET_HEADS)
    ret_c, ret_l = _retention_bidir(q_c, k_c, v_c, q_l, k_l, v_l, log_g)
    nkc, nvc = _heads(nk_c, NA_HEADS), _heads(nv_c, NA_HEADS)
    na_l = _na_latent(_heads(nq_l, NA_HEADS), _heads(nk_l, NA_HEADS), _heads(nv_l, NA_HEADS), nkc, nvc, rpb)
    ret_out_l = _merge(_head_rms(ret_l)).astype(hl.dtype) * jax.nn.silu(rg_l)
    out_l = jnp.concatenate([ret_out_l, _merge(na_l)], axis=-1) @ w_out
    out_c = None
    if need_ctx:
        ret_out_c = _merge(_head_rms(ret_c)).astype(hc.dtype) * jax.nn.silu(rg_c)
        na_c = _blocked_attention(_heads(nq_c, NA_HEADS), nkc, nvc, NA_HD ** -0.5)
        out_c = jnp.concatenate([ret_out_c, _merge(na_c)], axis=-1) @ w_out
    return out_c, out_l


def _mla_q(cq, gq, wuq, rope):
    q = _heads(_rms_norm(cq, gq) @ wuq, MLA_HEADS)
    q_nope, q_pe = q[..., :MLA_NOPE], q[..., MLA_NOPE:]
    if rope is not None:
        q_pe = _apply_rope(q_pe, rope)
    return jnp.concatenate([q_nope, q_pe], axis=-1)


def _mla_kv(ckv, kr, gkv, wukv, rope):
    kv = _heads(_rms_norm(ckv, gkv) @ wukv, MLA_HEADS)
    k_pe = kr[:, None]
    if rope is not None:
        k_pe = _apply_rope(k_pe, rope)
    b, h, n, _ = kv.shape
    k = jnp.concatenate([kv[..., :MLA_NOPE], jnp.broadcast_to(k_pe, (b, h, n, MLA_ROPE))], axis=-1)
    return k, kv[..., MLA_NOPE:]


def _conv_centred(x, w, bias):
    y = lax.conv_general_dilated(x, w[:, None, :].astype(x.dtype), (1,), [((LRU_CONV - 1) // 2, LRU_CONV // 2)],
                                 dimension_numbers=('NWC', 'WIO', 'NWC'), feature_group_count=x.shape[-1])
    return y + bias


def _lin_comb(l, r):
    return l[0] * r[0], r[0] * l[1] + r[1]


def _rglru_dir(x, wa, ba, wi, bi, lam, h0):
    b, n, ch = x.shape
    xb = x.reshape(b, n, LRU_BLOCKS, LRU_BD)
    r = jax.nn.sigmoid((jnp.einsum('bnkc,kcd->bnkd', xb, wa).reshape(b, n, ch) + ba).astype(jnp.float32))
    ig = jax.nn.sigmoid((jnp.einsum('bnkc,kcd->bnkd', xb, wi).reshape(b, n, ch) + bi).astype(jnp.float32))
    log_a = -LRU_C * r * jax.nn.softplus(-lam.astype(jnp.float32))
    a = jnp.exp(log_a)
    u = jnp.sqrt(-jnp.expm1(2.0 * log_a)) * ig * x.astype(jnp.float32)
    acc_a, acc_u = lax.associative_scan(_lin_comb, (a, u), axis=1)
    hseq = acc_a * h0[:, None, :] + acc_u
    return hseq, hseq[:, -1]


def _rglru_bidir(xc, xl, wa, ba, wi, bi, lam):
    h0 = jnp.zeros((xc.shape[0], xc.shape[-1]), jnp.float32)
    fl = lambda t: jnp.flip(t, axis=1)
    hc_f, sc_f = _rglru_dir(xc, wa[0], ba[0], wi[0], bi[0], lam[0], h0)
    hl_f, _ = _rglru_dir(xl, wa[0], ba[0], wi[0], bi[0], lam[0], sc_f)
    hc_b, sc_b = _rglru_dir(fl(xc), wa[1], ba[1], wi[1], bi[1], lam[1], h0)
    hl_b, _ = _rglru_dir(fl(xl), wa[1], ba[1], wi[1], bi[1], lam[1], sc_b)
    return hc_f + fl(hc_b), hl_f + fl(hl_b)


def _odd_mixer(hc, hl, w_in, gq, wuq, gkv, wukv, conv_w, conv_b, wa, ba, wi, bi, lam, w_out, rope_mla, need_ctx):
    cq_c, ckv_c, kr_c, lx_c, ly_c = _split(hc @ w_in, ODD_SPLITS)
    cq_l, ckv_l, kr_l, lx_l, ly_l = _split(hl @ w_in, ODD_SPLITS)
    scale = (MLA_NOPE + MLA_ROPE) ** -0.5
    k_c, v_c = _mla_kv(ckv_c, kr_c, gkv, wukv, None)
    k_l, v_l = _mla_kv(ckv_l, kr_l, gkv, wukv, rope_mla)
    q_l = _mla_q(cq_l, gq, wuq, rope_mla)
    att_l = _blocked_attention(q_l, jnp.concatenate([k_c, k_l], axis=2), jnp.concatenate([v_c, v_l], axis=2), scale)
    rc, rl = _rglru_bidir(_conv_centred(lx_c, conv_w, conv_b), _conv_centred(lx_l, conv_w, conv_b), wa, ba, wi, bi, lam)
    lru_l = rl.astype(hl.dtype) * jax.nn.gelu(ly_l)
    out_l = jnp.concatenate([_merge(att_l), lru_l], axis=-1) @ w_out
    out_c = None
    if need_ctx:
        att_c = _blocked_attention(_mla_q(cq_c, gq, wuq, None), k_c, v_c, scale)
        lru_c = rc.astype(hc.dtype) * jax.nn.gelu(ly_c)
        out_c = jnp.concatenate([_merge(att_c), lru_c], axis=-1) @ w_out
    return out_c, out_l


def _swiglu(h, wg, wu, wd):
    return (jax.nn.silu(h @ wg) * (h @ wu)) @ wd


def _moe(h, router, wg, wu, wd):
    logits = (h @ router).astype(jnp.float32)
    top_v, top_i = lax.top_k(logits, TOP_K)
    gates = jax.nn.softmax(top_v, axis=-1)
    combine = jnp.einsum('bnk,bnke->bne', gates, jax.nn.one_hot(top_i, N_EXPERTS, dtype=jnp.float32)).astype(h.dtype)
    out = jnp.zeros_like(h)
    for e in range(N_EXPERTS):
        out = out + combine[..., e:e + 1] * _swiglu(h, wg[e], wu[e], wd[e])
    return out


def setup_inputs(seed: int = 0) -> dict:
    key = jax.random.key(seed)
    ks = iter(jax.random.split(key, 32))
    f32 = jnp.float32

    def nrm(shape, scale):
        return jax.random.normal(next(ks), shape, f32) * scale

    D = D_MODEL
    ne, no = N_EVEN, N_ODD
    x = nrm((BATCH, SEQ, D), 1.0)
    c = nrm((BATCH, D), 1.0)
    ctx = nrm((BATCH, CTX_LEN, D), 1.0)
    c_ctx = nrm((D,), 1.0)
    w_mod = nrm((DEPTH, D, 6 * D), 0.5 * D ** -0.5)
    b_mod = nrm((DEPTH, 6 * D), 0.02)
    norm_g = 1.0 + nrm((DEPTH, 4, D), 0.05)
    w_in_even = nrm((ne, D, EVEN_IN), D ** -0.5)
    w_out_even = nrm((ne, EVEN_OUT, D), EVEN_OUT ** -0.5)
    m = jnp.arange(RET_HEADS, dtype=f32) + 5.0
    ret_base = jnp.log1p(-(2.0 ** -m)) + m * jnp.log(2.0)
    ret_decay = ret_base + nrm((ne, 2, RET_HEADS), 0.1)
    na_rpb = nrm((ne, NA_HEADS, 2 * NA_KH - 1, 2 * NA_KW - 1), 0.1)
    ffn_w_gate = nrm((ne, D, FFN_DENSE), D ** -0.5)
    ffn_w_up = nrm((ne, D, FFN_DENSE), D ** -0.5)
    ffn_w_down = nrm((ne, FFN_DENSE, D), FFN_DENSE ** -0.5)
    w_in_odd = nrm((no, D, ODD_IN), D ** -0.5)
    mla_q_norm = 1.0 + nrm((no, MLA_Q_RANK), 0.05)
    mla_w_uq = nrm((no, MLA_Q_RANK, MLA_HEADS * (MLA_NOPE + MLA_ROPE)), MLA_Q_RANK ** -0.5)
    mla_kv_norm = 1.0 + nrm((no, MLA_KV_RANK), 0.05)
    mla_w_ukv = nrm((no, MLA_KV_RANK, MLA_HEADS * (MLA_NOPE + MLA_V)), MLA_KV_RANK ** -0.5)
    lru_conv_w = nrm((no, LRU_CONV, LRU_WIDTH), LRU_CONV ** -0.5)
    lru_conv_b = nrm((no, LRU_WIDTH), 0.02)
    lru_w_a = nrm((no, 2, LRU_BLOCKS, LRU_BD, LRU_BD), LRU_BD ** -0.5)
    lru_b_a = nrm((no, 2, LRU_WIDTH), 0.02)
    lru_w_i = nrm((no, 2, LRU_BLOCKS, LRU_BD, LRU_BD), LRU_BD ** -0.5)
    lru_b_i = nrm((no, 2, LRU_WIDTH), 0.02)
    u = jax.random.uniform(next(ks), (no, 2, LRU_WIDTH), f32, 0.9, 0.999)
    sig = u ** (1.0 / LRU_C)
    lru_lambda = jnp.log(sig) - jnp.log1p(-sig)
    w_out_odd = nrm((no, ODD_OUT, D), ODD_OUT ** -0.5)
    router_w = nrm((no, D, N_EXPERTS), D ** -0.5)
    moe_w_gate = nrm((no, N_EXPERTS, D, FFN_EXPERT), D ** -0.5)
    moe_w_up = nrm((no, N_EXPERTS, D, FFN_EXPERT), D ** -0.5)
    moe_w_down = nrm((no, N_EXPERTS, FFN_EXPERT, D), FFN_EXPERT ** -0.5)
    return {'x': x, 'c': c, 'ctx': ctx, 'c_ctx': c_ctx, 'w_mod': w_mod, 'b_mod': b_mod, 'norm_g': norm_g,
            'w_in_even': w_in_even, 'w_out_even': w_out_even, 'ret_decay': ret_decay, 'na_rpb': na_rpb,
            'ffn_w_gate': ffn_w_gate, 'ffn_w_up': ffn_w_up, 'ffn_w_down': ffn_w_down,
            'w_in_odd': w_in_odd, 'mla_q_norm': mla_q_norm, 'mla_w_uq': mla_w_uq, 'mla_kv_norm': mla_kv_norm,
            'mla_w_ukv': mla_w_ukv, 'lru_conv_w': lru_conv_w, 'lru_conv_b': lru_conv_b, 'lru_w_a': lru_w_a,
            'lru_b_a': lru_b_a, 'lru_w_i': lru_w_i, 'lru_b_i': lru_b_i, 'lru_lambda': lru_lambda,
            'w_out_odd': w_out_odd, 'router_w': router_w, 'moe_w_gate': moe_w_gate, 'moe_w_up': moe_w_up,
            'moe_w_down': moe_w_down}


def reference(x, c, ctx, c_ctx, w_mod, b_mod, norm_g, w_in_even, w_out_even, ret_decay, na_rpb,
              ffn_w_gate, ffn_w_up, ffn_w_down, w_in_odd, mla_q_norm, mla_w_uq, mla_kv_norm, mla_w_ukv,
              lru_conv_w, lru_conv_b, lru_w_a, lru_b_a, lru_w_i, lru_b_i, lru_lambda, w_out_odd,
              router_w, moe_w_gate, moe_w_up, moe_w_down):
    b, s, d = x.shape
    rope_ret = _axial_rope(s, RET_DK)
    rope_mla = _axial_rope(s, MLA_ROPE)
    sc = jax.nn.silu(c)
    scc = jax.nn.silu(c_ctx)
    xl, xc = x, ctx
    for i in range(DEPTH):
        need_ctx = i < DEPTH - 1
        j = i // 2
        ml = (sc @ w_mod[i] + b_mod[i]).reshape(b, 6, 1, d)
        mc = (scc @ w_mod[i] + b_mod[i]).reshape(6, d)
        g = norm_g[i]
        hl = _rms_norm(xl, g[0]) * (1.0 + ml[:, 0]) + ml[:, 1]
        hc = _rms_norm(xc, g[0]) * (1.0 + mc[0]) + mc[1]
        if i % 2 == 0:
            oc, ol = _even_mixer(hc, hl, w_in_even[j], w_out_even[j], ret_decay[j], na_rpb[j], rope_ret, need_ctx)
            ffn = lambda h: _swiglu(h, ffn_w_gate[j], ffn_w_up[j], ffn_w_down[j])
        else:
            oc, ol = _odd_mixer(hc, hl, w_in_odd[j], mla_q_norm[j], mla_w_uq[j], mla_kv_norm[j], mla_w_ukv[j],
                                lru_conv_w[j], lru_conv_b[j], lru_w_a[j], lru_b_a[j], lru_w_i[j], lru_b_i[j],
                                lru_lambda[j], w_out_odd[j], rope_mla, need_ctx)
            ffn = lambda h: _moe(h, router_w[j], moe_w_gate[j], moe_w_up[j], moe_w_down[j])
        xl = xl + ml[:, 2] * _rms_norm(ol, g[1])
        hl = _rms_norm(xl, g[2]) * (1.0 + ml[:, 3]) + ml[:, 4]
        xl = xl + ml[:, 5] * _rms_norm(ffn(hl), g[3])
        if need_ctx:
            xc = xc + mc[2] * _rms_norm(oc, g[1])
            hc = _rms_norm(xc, g[2]) * (1.0 + mc[3]) + mc[4]
            xc = xc + mc[5] * _rms_norm(ffn(hc), g[3])
    return xl
```

```python
import numpy as np
import concourse.bass as bass
import concourse.mybir as mybir
from concourse.bass_utils import run_bass_kernel_spmd
from contextlib import ExitStack

F32 = mybir.dt.float32
BF16 = mybir.dt.bfloat16
AF = mybir.ActivationFunctionType
ALU = mybir.AluOpType
AX = mybir.AxisListType


class Prog:
    ENG = ('pe', 'act', 'dve', 'pool', 'sp')
    NDMA = 8

    def __init__(self, nc, stack):
        self.nc = nc
        self.stack = stack
        self.ops = {e: [] for e in self.ENG}
        self.sems = {}
        for e in self.ENG:
            self.sems[('e', e)] = stack.enter_context(nc.semaphore('s_' + e))
        self.cnt = {e: 0 for e in self.ENG}
        self.dq = ('sp', 'pool', 'act')
        for q in self.dq:
            for i in range(self.NDMA):
                self.sems[('d', q, i)] = stack.enter_context(nc.semaphore(f'd_{q}{i}'))
        self.dcnt = {q: 0 for q in self.dq}
        self.dval = {q: [0] * self.NDMA for q in self.dq}
        self.waited = {e: {} for e in self.ENG}
        self.res = {}
        self.n_wait = 0
        self.in_loop = False

    def sb(self, name, shape, dt):
        self.nalloc = getattr(self, 'nalloc', 0) + 1
        return self.stack.enter_context(self.nc.sbuf_tensor(f"sb{self.nalloc}_{name}", list(shape), dt))

    def ps(self, name, shape, dt=F32):
        return self.stack.enter_context(self.nc.psum_tensor("ps_" + name, list(shape), dt))

    def _wait(self, eng, tok):
        key, val = tok
        if key == ('e', 'pe') and eng == 'pe':
            return
        if self.waited[eng].get(key, 0) >= val:
            return
        self.waited[eng][key] = val
        self.ops[eng].append(('wait', key, val))
        self.n_wait += 1

    def _deps(self, eng, reads, writes):
        for r in reads:
            st = self.res.get(r)
            if st is not None and st[0] is not None:
                self._wait(eng, st[0])
        for w in writes:
            st = self.res.get(w)
            if st is not None:
                if st[0] is not None:
                    self._wait(eng, st[0])
                for k, v in st[1].items():
                    self._wait(eng, (k, v))

    def _update(self, tok, reads, writes):
        for r in reads:
            st = self.res.get(r)
            if st is None:
                st = [None, {}]
                self.res[r] = st
            if st[1].get(tok[0], 0) < tok[1]:
                st[1][tok[0]] = tok[1]
        for w in writes:
            self.res[w] = [tok, {}]

    def op(self, eng, fn, reads=(), writes=()):
        self._deps(eng, reads, writes)
        self.cnt[eng] += 1
        tok = (('e', eng), self.cnt[eng])
        self.ops[eng].append(('op', fn))
        self._update(tok, reads, writes)
        return tok

    def dma(self, q, out, in_, reads=(), writes=(), dyn=None):
        self._deps(q, reads, writes)
        i = self.dcnt[q] % self.NDMA
        self.dcnt[q] += 1
        v = self.dval[q][i]
        key = ('d', q, i)
        if v > 0:
            self._wait(q, (key, v))
        self.dval[q][i] = v + 16
        tok = (key, v + 16)
        self.ops[q].append(('dma', out, in_, key, dyn))
        self._update(tok, reads, writes)
        return tok

    def barrier(self):
        for e in self.ENG:
            for k, sem in self.sems.items():
                if k[0] == 'e':
                    v = self.cnt[k[1]]
                    if k[1] == e:
                        continue
                else:
                    v = self.dval[k[1]][k[2]]
                if v > 0 and self.waited[e].get(k, 0) < v:
                    self.waited[e][k] = v
                    self.ops[e].append(('wait', k, v))
        self.res = {}

    def _cur(self, k):
        return self.cnt[k[1]] if k[0] == 'e' else self.dval[k[1]][k[2]]

    def loop(self, n, body):
        assert not self.in_loop
        self.barrier()
        self.in_loop = True
        v0 = {k: self._cur(k) for k in self.sems}
        start = {e: len(self.ops[e]) for e in self.ENG}
        for q in self.dq:
            self.dcnt[q] = 0
        body()
        vA = {k: self._cur(k) for k in self.sems}
        for e in self.ENG:
            del self.ops[e][start[e]:]
        for e in self.ENG:
            self.waited[e] = dict(v0)
            self.waited[e].pop(('e', e), None)
        for q in self.dq:
            self.dcnt[q] = 0
        body()
        vB = {k: self._cur(k) for k in self.sems}
        stride = {k: vB[k] - vA[k] for k in self.sems}
        for k in self.sems:
            assert vA[k] - v0[k] == stride[k], (k, v0[k], vA[k], vB[k])
        for e in self.ENG:
            bops = self.ops[e][start[e]:]
            del self.ops[e][start[e]:]
            if bops:
                self.ops[e].append(('loop', n, bops, stride))
        for k in self.sems:
            fin = v0[k] + n * stride[k]
            if k[0] == 'e':
                self.cnt[k[1]] = fin
            else:
                self.dval[k[1]][k[2]] = fin
        for e in self.ENG:
            self.waited[e] = dict(v0)
            self.waited[e].pop(('e', e), None)
        self.in_loop = False
        self.barrier()

    def finish(self):
        for q in self.dq:
            for i in range(self.NDMA):
                if self.dval[q][i] > 0:
                    self._wait('sp', (('d', q, i), self.dval[q][i]))
        for e in ('pe', 'act', 'dve', 'pool'):
            if self.cnt[e] > 0:
                self._wait('sp', (('e', e), self.cnt[e]))

    def flush(self, final=False):
        nc = self.nc
        self.barrier()
        if final:
            self.finish()
        sems = self.sems

        def replay(ename, e):
            own = sems[('e', ename)]
            for o in self.ops[ename]:
                if o[0] == 'wait':
                    e.wait_ge(sems[o[1]], o[2])
                elif o[0] == 'op':
                    o[1](e, None).then_inc(own, 1)
                else:
                    e.dma_start(out=o[1], in_=o[2]).then_inc(sems[o[3]], 16)

        with nc.Block() as block:
            @block.tensor
            def _(e):
                replay('pe', e)

            @block.scalar
            def _(e):
                replay('act', e)

            @block.vector
            def _(e):
                replay('dve', e)

            @block.gpsimd
            def _(e):
                replay('pool', e)

            @block.sync
            def _(e):
                replay('sp', e)
        self.n_instr = getattr(self, 'n_instr', 0) + sum(len(v) for v in self.ops.values())
        self.ops = {e: [] for e in self.ENG}

    def emit(self):
        self.flush(final=True)


D = 4096
KC = D // 128
EPS = 1e-6
SEQ = 4096
LCTX = 256
NTOK = SEQ + LCTX
NOWN = SEQ // 2 + LCTX // 2
ALL_BLOCKS = [(0, 1024), (1024, 1024), (2048, 1024), (3072, 1024), (4096, 256)]
OWN_BLOCKS_H = [(0, 1024), (1024, 1024), (4096, 128)]
OWN_BLOCKS_O = [(0, 1024), (1024, 1024), (2048, 128)]


def _new_nc():
    return bass.Bass("TRN2", target_bir_lowering=False)


def _subs(tl):
    return [(s, min(512, tl - s)) for s in range(0, tl, 512)]


class Ctx:
    def __init__(self, nc, st):
        self.nc = nc
        self.P = Prog(nc, st)
        self.banks = [self.P.ps(f"bank{i}", [128, 512]) for i in range(8)]
        self.rr = 0

    def scratch(self, name, shape, dt):
        return self.nc.dram_tensor(name, list(shape), dt, kind="Internal").ap()

    def alt(self):
        self.rr += 1
        return 'act' if self.rr % 2 else 'dve'


def copy_op(C, eng, out, in_, reads, writes):
    P = C.P
    if eng == 'act':
        P.op('act', lambda e, i: e.copy(out=out, in_=in_), reads=reads, writes=writes)
    elif eng == 'dve':
        P.op('dve', lambda e, i: e.tensor_copy(out=out, in_=in_), reads=reads, writes=writes)
    else:
        P.op('pool', lambda e, i: e.tensor_copy(out=out, in_=in_), reads=reads, writes=writes)


def lin(C, st, xT, K, blocks, Ws, ncols, mode, evac, tag, ntile=4, tb=1024, bufs=None):
    P = C.P
    kc_n = K // 128
    nW = len(Ws)
    cw = 128 * ntile
    NWB = 6
    if bufs is None:
        xs = P.sb(f"xs_{tag}", [128, kc_n, tb], BF16)
        wb = [[P.sb(f"wb_{tag}{m}_{i}", [128, cw], BF16) for i in range(NWB)] for m in range(nW)]
    else:
        xs, wb = bufs
    wi = 0
    for (t0, tl) in blocks:
        for q0 in range(0, kc_n, 8):
            q1 = min(kc_n, q0 + 8)
            P.dma('sp', xs[:, q0:q1, :tl], xT[q0 * 128:q1 * 128, t0:t0 + tl].rearrange("(c p) t -> p c t", p=128),
                  writes=[('xs', kc) for kc in range(q0, q1)])
        subs = _subs(tl)
        mts = [(m * 128, min(128, tl - m * 128)) for m in range((tl + 127) // 128)]
        for c0 in range(0, ncols, cw):
            ncb = min(cw, ncols - c0)
            nj = ncb // 128
            for kc in range(kc_n):
                w = wi % NWB
                wi += 1
                for m in range(nW):
                    P.dma('pool', wb[m][w][:, :ncb], Ws[m][kc * 128:(kc + 1) * 128, c0:c0 + ncb], writes=[('wb', m, w)])
                if mode == 'fm':
                    for m in range(nW):
                        for j in range(nj):
                            for si, (s, sl) in enumerate(subs):
                                b = (m * nj + j) * len(subs) + si
                                P.op('pe', lambda e, i, b=b, w=w, m=m, j=j, kc=kc, s=s, sl=sl: e.matmul(
                                    C.banks[b][:, :sl], lhsT=wb[m][w][:, j * 128:(j + 1) * 128], rhs=xs[:, kc, s:s + sl],
                                    start=(kc == 0), stop=(kc == kc_n - 1)),
                                    reads=[('wb', m, w), ('xs', kc)], writes=[('bank', b)])
                else:
                    for mi, (ms, ml) in enumerate(mts):
                        P.op('pe', lambda e, i, mi=mi, w=w, kc=kc, ms=ms, ml=ml, ncb=ncb: e.matmul(
                            C.banks[mi][:ml, :ncb], lhsT=xs[:, kc, ms:ms + ml], rhs=wb[0][w][:, :ncb],
                            start=(kc == 0), stop=(kc == kc_n - 1)),
                            reads=[('wb', 0, w), ('xs', kc)], writes=[('bank', mi)])
            if mode == 'fm':
                for j in range(nj):
                    for si, (s, sl) in enumerate(subs):
                        bl = [(m * nj + j) * len(subs) + si for m in range(nW)]
                        evac(j, si, c0 + j * 128, t0, s, sl, bl)
            else:
                for mi, (ms, ml) in enumerate(mts):
                    evac(mi, c0, t0, ms, ml, mi, ncb)


def rms_bcast(C, R, src, tl, nch, rkey, div, skeys):
    P = C.P
    sb_ = C.banks[7]
    for kc in range(nch):
        s = R['sq'][kc % 3]
        P.op('act', lambda e, i, s=s, kc=kc: e.activation(out=s[:, :tl], in_=src[:, kc, :tl], func=AF.Square),
             reads=[skeys(kc)], writes=[('sq', kc % 3)])
        P.op('pe', lambda e, i, s=s, kc=kc: e.matmul(sb_[:, :tl], lhsT=R['ones'][:, :], rhs=s[:, :tl],
                                                     start=(kc == 0), stop=(kc == nch - 1)),
             reads=[('sq', kc % 3), 'ones'], writes=[('bank', 7)])
    rb = R['rb']
    P.op('act', lambda e, i: e.activation(out=rb[:, :tl], in_=sb_[:, :tl], func=AF.Sqrt, bias=R['epsb'][:, 0:1], scale=1.0 / div),
         reads=[('bank', 7), 'eps'], writes=[rkey])
    P.op('dve', lambda e, i: e.reciprocal(out=rb[:, :tl], in_=rb[:, :tl]), reads=[rkey], writes=[rkey])


def load_consts(C, vec_ap):
    P = C.P
    R = {}
    R['vec'] = P.sb("vec", [128, KC, 16], F32)
    R['A'] = P.sb("Avec", [128, KC, 8], F32)
    R['ones'] = P.sb("ones", [128, 128], F32)
    R['onesb'] = P.sb("onesb", [128, 128], BF16)
    R['epsb'] = P.sb("epsb", [128, 1], F32)
    R['sq'] = [P.sb(f"sq{i}", [128, 512], F32) for i in range(3)]
    R['tmp'] = [P.sb(f"tmp{i}", [128, 512], F32) for i in range(3)]
    R['rb'] = P.sb("rb", [128, 512], F32)
    P.dma('sp', R['vec'][:, :, :], vec_ap[:, :, :], writes=['vec'])
    P.op('pool', lambda e, i: e.memset(R['ones'][:, :], 1.0), writes=['ones'])
    P.op('pool', lambda e, i: e.memset(R['onesb'][:, :], 1.0), writes=['onesb'])
    P.op('pool', lambda e, i: e.memset(R['epsb'][:, :], EPS), writes=['eps'])
    v, A = R['vec'], R['A']
    for k in range(2):
        o = 6 * k
        P.op('dve', lambda e, i, k=k, o=o: e.scalar_tensor_tensor(out=A[:, :, k], in0=v[:, :, o + 0], scalar=1.0, in1=v[:, :, 12], op0=ALU.add, op1=ALU.mult),
             reads=['vec'], writes=[('A', k)])
        P.op('dve', lambda e, i, k=k, o=o: e.tensor_tensor(out=A[:, :, 2 + k], in0=v[:, :, o + 2], in1=v[:, :, 13], op=ALU.mult),
             reads=['vec'], writes=[('A', 2 + k)])
        P.op('dve', lambda e, i, k=k, o=o: e.scalar_tensor_tensor(out=A[:, :, 4 + k], in0=v[:, :, o + 3], scalar=1.0, in1=v[:, :, 14], op0=ALU.add, op1=ALU.mult),
             reads=['vec'], writes=[('A', 4 + k)])
        P.op('dve', lambda e, i, k=k, o=o: e.tensor_tensor(out=A[:, :, 6 + k], in0=v[:, :, o + 5], in1=v[:, :, 15], op=ALU.mult),
             reads=['vec'], writes=[('A', 6 + k)])
    C.P.flush()
    return R


def stage_norm_in(C, R, xT, hT, blocks):
    P = C.P
    with ExitStack() as st:
        P.stack, old = st, P.stack
        xs = [P.sb(f"nx{i}", [128, KC, 512], F32) for i in range(2)]
        hs = P.sb("nh", [128, KC, 512], BF16)
        xv = xT.rearrange("(c p) t -> p c t", p=128)
        hv = hT.rearrange("(c p) t -> p c t", p=128)
        for bi, (t0, tl, k) in enumerate(blocks):
            xb = xs[bi % 2]
            for q in range(4):
                P.dma('sp' if q % 2 == 0 else 'act', xb[:, q * 8:(q + 1) * 8, :tl], xv[:, q * 8:(q + 1) * 8, t0:t0 + tl],
                      writes=[('x', bi % 2, q)])
            rms_bcast(C, R, xb, tl, KC, 'rb', D, lambda kc, bi=bi: ('x', bi % 2, kc // 8))
            for kc in range(KC):
                t = R['tmp'][kc % 3]
                P.op('dve', lambda e, i, t=t, kc=kc, xb=xb, tl=tl: e.tensor_tensor(out=t[:, :tl], in0=xb[:, kc, :tl], in1=R['rb'][:, :tl], op=ALU.mult),
                     reads=[('x', bi % 2, kc // 8), 'rb'], writes=[('tmp', kc % 3)])
                P.op('act', lambda e, i, t=t, kc=kc, k=k, tl=tl: e.activation(out=hs[:, kc, :tl], in_=t[:, :tl], func=AF.Identity,
                                                                             scale=R['A'][:, kc, k:k + 1], bias=R['vec'][:, kc, 6 * k + 1:6 * k + 2]),
                     reads=[('tmp', kc % 3)], writes=[('h', kc // 8)])
            for q in range(4):
                P.dma('pool', hv[:, q * 8:(q + 1) * 8, t0:t0 + tl], hs[:, q * 8:(q + 1) * 8, :tl], reads=[('h', q)])
        P.flush()
        P.stack = old


from contextlib import contextmanager


@contextmanager
def stage(C):
    with ExitStack() as st:
        old = C.P.stack
        C.P.stack = st
        yield
        C.P.flush()
        C.P.stack = old


def compute_loggamma(C, R, rdec_ap):
    P = C.P
    lg = P.sb("lg", [128, 16], F32)
    R['lg'] = lg
    R['lnks'] = P.sb("lnks", [128, 1], F32)
    P.dma('sp', lg[:, :], rdec_ap[:, :], writes=['lg'])
    P.op('pool', lambda e, i: e.memset(R['lnks'][:, :], float(np.log(1.0 / 16.0))), writes=['lnks'])
    P.op('act', lambda e, i: e.activation(out=lg[:, :], in_=lg[:, :], func=AF.Exp, scale=-1.0), reads=['lg'], writes=['lg'])
    P.op('dve', lambda e, i: e.tensor_scalar(out=lg[:, :], in0=lg[:, :], scalar1=1.0, scalar2=None, op0=ALU.add), reads=['lg'], writes=['lg'])
    P.op('act', lambda e, i: e.activation(out=lg[:, :], in_=lg[:, :], func=AF.Ln), reads=['lg'], writes=['lg'])
    P.op('dve', lambda e, i: e.tensor_scalar(out=lg[:, :], in0=lg[:, :], scalar1=-1.0, scalar2=None, op0=ALU.mult), reads=['lg'], writes=['lg'])
    P.flush()


def inproj_rope(C, R, hT, W, col0, blocks, opos, etab, ropetab, outF, outB, is_q, tag):
    P = C.P
    with stage(C):
        ncol = 1280 if is_q else 1024
        e0 = 0 if is_q else 1280
        et = P.sb("et", [128, ncol], F32)
        P.dma('sp', et[:, :], etab[:, e0:e0 + ncol], writes=['et'])
        TB_ = [P.sb(f"tab{h}", [128, ncol], F32) for h in range(8)]
        half = ncol // 2
        for h in range(8):
            for d in range(2):
                if is_q:
                    segs = [(d * 512, 512), (1024 + d * 128, 128)]
                else:
                    segs = [(d * 512, 512)]
                for (a, n) in segs:
                    if is_q:
                        P.op('act', lambda e, i, h=h, d=d, a=a, n=n: e.activation(out=TB_[h][:, a:a + n], in_=et[:, a:a + n], func=AF.Exp,
                                                                                  scale=R['lg'][:, d * 8 + h:d * 8 + h + 1]),
                             reads=['et', 'lg'], writes=[('tab', h, a)])
                    else:
                        P.op('act', lambda e, i, h=h, d=d, a=a, n=n: e.activation(out=TB_[h][:, a:a + n], in_=et[:, a:a + n], func=AF.Exp,
                                                                                  scale=R['lg'][:, d * 8 + h:d * 8 + h + 1], bias=R['lnks'][:, 0:1]),
                             reads=['et', 'lg', 'lnks'], writes=[('tab', h, a)])
        NB = 3
        q32 = [P.sb(f"q32_{i}", [128, 512], F32) for i in range(NB)]
        qsw = [P.sb(f"qsw_{i}", [128, 512], F32) for i in range(NB)]
        cs = [P.sb(f"cs_{i}", [128, 2, 512], F32) for i in range(NB)]
        of = [P.sb(f"of_{i}", [128, 512], BF16) for i in range(NB)]
        ob = [P.sb(f"ob_{i}", [128, 512], BF16) for i in range(NB)]
        cnt = [0]

        def evac(j, si, c, t0, s, sl, bl):
            n = cnt[0] % NB
            cnt[0] += 1
            ch = (c - col0) // 128
            h, ctype = ch // 2, ch % 2
            isctx = t0 >= SEQ
            pos = t0 + s
            b = bl[0]
            P.op('act', lambda e, i: e.copy(out=q32[n][:, :sl], in_=C.banks[b][:, :sl]), reads=[('bank', b)], writes=[('q32', n)])
            src = q32[n]
            if not isctx:
                P.dma('act', qsw[n][0:64, :sl], q32[n][64:128, :sl], reads=[('q32', n)], writes=[('qswa', n)])
                P.dma('act', qsw[n][64:128, :sl], q32[n][0:64, :sl], reads=[('q32', n)], writes=[('qswb', n)])
                P.dma('sp', cs[n][:, :, :sl], ropetab[ctype, :, :, pos:pos + sl].rearrange("k p t -> p k t"), writes=[('cs', n)])
                P.op('dve', lambda e, i: e.tensor_tensor(out=q32[n][:, :sl], in0=q32[n][:, :sl], in1=cs[n][:, 0, :sl], op=ALU.mult),
                     reads=[('q32', n), ('cs', n)], writes=[('q32', n)])
                P.op('dve', lambda e, i: e.tensor_tensor(out=qsw[n][:, :sl], in0=qsw[n][:, :sl], in1=cs[n][:, 1, :sl], op=ALU.mult),
                     reads=[('qswa', n), ('qswb', n), ('cs', n)], writes=[('qswa', n), ('qswb', n)])
                P.op('dve', lambda e, i: e.tensor_tensor(out=q32[n][:, :sl], in0=q32[n][:, :sl], in1=qsw[n][:, :sl], op=ALU.add),
                     reads=[('q32', n), ('qswa', n), ('qswb', n)], writes=[('q32', n)])
            if is_q:
                if isctx:
                    ta, tb2 = 1024, 1024 + 128
                else:
                    ta, tb2 = 0, 512
            else:
                ta, tb2 = 0, 512
            o0 = opos(t0) + s
            P.op('dve', lambda e, i: e.tensor_tensor(out=of[n][:, :sl], in0=src[:, :sl], in1=TB_[h][:, ta:ta + sl], op=ALU.mult),
                 reads=[('q32', n), ('tab', h, ta)], writes=[('of', n)])
            P.op('dve', lambda e, i: e.tensor_tensor(out=ob[n][:, :sl], in0=src[:, :sl], in1=TB_[h][:, tb2:tb2 + sl], op=ALU.mult),
                 reads=[('q32', n), ('tab', h, tb2)], writes=[('ob', n)])
            r0 = ch * 128
            P.dma('sp', outF[r0:r0 + 128, o0:o0 + sl], of[n][:, :sl], reads=[('of', n)])
            P.dma('sp', outB[r0:r0 + 128, o0:o0 + sl], ob[n][:, :sl], reads=[('ob', n)])

        lin(C, None, hT, D, blocks, [W[:, col0:col0 + 2048]], 2048, 'fm',
            lambda j, si, c, t0, s, sl, bl: evac(j, si, c + col0, t0, s, sl, bl), tag)


def inproj_plain(C, R, hT, W, col0, blocks, opos, out, mode, tag, func=None):
    P = C.P
    with stage(C):
        NB = 4
        ob = [P.sb(f"pob_{i}", [128, 512], BF16) for i in range(NB)]
        cnt = [0]

        def evac_fm(j, si, c, t0, s, sl, bl):
            n = cnt[0] % NB
            cnt[0] += 1
            b = bl[0]
            if func is not None:
                P.op('act', lambda e, i: e.activation(out=ob[n][:, :sl], in_=C.banks[b][:, :sl], func=func), reads=[('bank', b)], writes=[('ob', n)])
            else:
                copy_op(C, C.alt(), ob[n][:, :sl], C.banks[b][:, :sl], [('bank', b)], [('ob', n)])
            o0 = opos(t0) + s
            P.dma('sp', out[c:c + 128, o0:o0 + sl], ob[n][:, :sl], reads=[('ob', n)])

        def evac_tm(mi, c0, t0, ms, ml, b, ncb):
            n = cnt[0] % NB
            cnt[0] += 1
            copy_op(C, C.alt(), ob[n][:ml, :ncb], C.banks[b][:ml, :ncb], [('bank', b)], [('ob', n)])
            o0 = opos(t0) + ms
            P.dma('sp', out[o0:o0 + ml, c0:c0 + ncb], ob[n][:ml, :ncb], reads=[('ob', n)])

        lin(C, None, hT, D, blocks, [W[:, col0:col0 + 2048]], 2048, mode, evac_fm if mode == 'fm' else evac_tm, tag)


def stage_retention(C, R, QF, QB, KF, KB, RV, RG, CAT, nexp_ap, cm_ap):
    P = C.P
    with stage(C):
        nx = P.sb("nexp", [128, 2, 170], F32)
        cm = P.sb("cm", [128, 8, 512], F32)
        sc = P.sb("sc", [128, 2, 170], F32)
        P.dma('sp', nx[:, :, :], nexp_ap[:, :, :], writes=['nx'])
        P.dma('sp', cm[:, :, :], cm_ap[:, :, :], writes=['cm'])
        Kt = [P.sb(f"rK{d}", [128, 2, NTOK], BF16) for d in range(2)]
        Vt = P.sb("rV", [128, 34, 256], BF16)
        Qt = [[P.sb(f"rQ{i}_{d}", [128, 2, 512], BF16) for d in range(2)] for i in range(2)]
        Gt = [P.sb(f"rG{i}", [128, 2, 512], BF16) for i in range(2)]
        Pt = [P.sb(f"rP{i}", [128, 512], BF16) for i in range(3)]
        ot = [P.sb(f"ro{i}", [128, 512], BF16) for i in range(2)]
        Qs, Ks = [QF, QB], [KF, KB]
        it = 0
        for h in range(8):
            for d in range(2):
                P.op('act', lambda e, i, d=d, h=h: e.activation(out=sc[:, d, :], in_=nx[:, d, :], func=AF.Exp, scale=R['lg'][:, d * 8 + h:d * 8 + h + 1]),
                     reads=['nx', 'lg'], writes=[('sc', d)])
                P.dma('sp', Kt[d][:, :, :], Ks[d][h * 256:(h + 1) * 256, :].rearrange("(c p) t -> p c t", p=128), writes=[('K', d)])
            P.dma('sp', Vt[:, :, :], RV[:, h * 256:(h + 1) * 256].rearrange("(k p) d -> p k d", p=128), writes=['V'])
            for ti in range(5):
                W = 512 if ti < 4 else 128
                o0 = ti * 512 if ti < 4 else 2048
                par = it % 2
                it += 1
                for d in range(2):
                    P.dma('act', Qt[par][d][:, :, :W], Qs[d][h * 256:(h + 1) * 256, o0:o0 + W].rearrange("(c p) t -> p c t", p=128),
                          writes=[('Q', par, d)])
                P.dma('act', Gt[par][:, :, :W], RG[h * 256:(h + 1) * 256, o0:o0 + W].rearrange("(c p) t -> p c t", p=128), writes=[('G', par)])
                pairs = []
                for d in range(2):
                    if ti < 4:
                        kbs = list(range(34)) if d == 0 else list(range(16)) + [32, 33]
                    else:
                        kbs = [32, 33] if d == 0 else [32]
                    pairs += [(d, kb) for kb in kbs]
                for pi, (d, kb) in enumerate(pairs):
                    sb_i = pi % 2
                    for c in range(2):
                        P.op('pe', lambda e, i, sb_i=sb_i, d=d, c=c, kb=kb, par=par, W=W: e.matmul(
                            C.banks[sb_i][:, :W], lhsT=Kt[d][:, c, kb * 128:(kb + 1) * 128], rhs=Qt[par][d][:, c, :W], start=(c == 0), stop=(c == 1)),
                            reads=[('K', d), ('Q', par, d)], writes=[('bank', sb_i)])
                    diag = (ti < 4 and 4 * ti <= kb < 4 * ti + 4) or (ti == 4 and kb == 32)
                    r = kb - 4 * ti if ti < 4 else 0
                    pn = pi % 3
                    col = ti * 34 + kb
                    if diag:
                        P.op('dve', lambda e, i, pn=pn, sb_i=sb_i, d=d, col=col, r=r, W=W: e.scalar_tensor_tensor(
                            out=Pt[pn][:, :W], in0=C.banks[sb_i][:, :W], scalar=sc[:, d, col:col + 1], in1=cm[:, d * 4 + r, :W], op0=ALU.mult, op1=ALU.mult),
                            reads=[('bank', sb_i), ('sc', d), 'cm'], writes=[('P', pn)])
                    elif pi % 2 == 0:
                        P.op('act', lambda e, i, pn=pn, sb_i=sb_i, d=d, col=col, W=W: e.activation(
                            out=Pt[pn][:, :W], in_=C.banks[sb_i][:, :W], func=AF.Identity, scale=sc[:, d, col:col + 1]),
                            reads=[('bank', sb_i), ('sc', d)], writes=[('P', pn)])
                    else:
                        P.op('dve', lambda e, i, pn=pn, sb_i=sb_i, d=d, col=col, W=W: e.tensor_scalar(
                            out=Pt[pn][:, :W], in0=C.banks[sb_i][:, :W], scalar1=sc[:, d, col:col + 1], scalar2=None, op0=ALU.mult),
                            reads=[('bank', sb_i), ('sc', d)], writes=[('P', pn)])
                    for dv in range(2):
                        P.op('pe', lambda e, i, dv=dv, kb=kb, pn=pn, pi=pi, W=W, last=(pi == len(pairs) - 1): e.matmul(
                            C.banks[2 + dv][:, :W], lhsT=Vt[:, kb, dv * 128:(dv + 1) * 128], rhs=Pt[pn][:, :W], start=(pi == 0), stop=last),
                            reads=['V', ('P', pn)], writes=[('bank', 2 + dv)])
                for dv in range(2):
                    s = R['sq'][dv]
                    P.op('act', lambda e, i, s=s, dv=dv, W=W: e.activation(out=s[:, :W], in_=C.banks[2 + dv][:, :W], func=AF.Square),
                         reads=[('bank', 2 + dv)], writes=[('sq', dv)])
                    P.op('pe', lambda e, i, s=s, dv=dv, W=W: e.matmul(C.banks[7][:, :W], lhsT=R['ones'][:, :], rhs=s[:, :W], start=(dv == 0), stop=(dv == 1)),
                         reads=[('sq', dv), 'ones'], writes=[('bank', 7)])
                rb = R['rb']
                P.op('act', lambda e, i, W=W: e.activation(out=rb[:, :W], in_=C.banks[7][:, :W], func=AF.Sqrt, bias=R['epsb'][:, 0:1], scale=1.0 / 256),
                     reads=[('bank', 7), 'eps'], writes=['rb'])
                P.op('dve', lambda e, i, W=W: e.reciprocal(out=rb[:, :W], in_=rb[:, :W]), reads=['rb'], writes=['rb'])
                for dv in range(2):
                    t = R['tmp'][dv]
                    P.op('dve', lambda e, i, t=t, dv=dv, W=W: e.tensor_tensor(out=t[:, :W], in0=C.banks[2 + dv][:, :W], in1=rb[:, :W], op=ALU.mult),
                         reads=[('bank', 2 + dv), 'rb'], writes=[('tmp', dv)])
                    P.op('dve', lambda e, i, t=t, dv=dv, W=W, par=par: e.tensor_tensor(out=ot[dv][:, :W], in0=t[:, :W], in1=Gt[par][:, dv, :W], op=ALU.mult),
                         reads=[('tmp', dv), ('G', par)], writes=[('ot', dv)])
                    r0 = h * 256 + dv * 128
                    P.dma('sp', CAT[r0:r0 + 128, o0:o0 + W], ot[dv][:, :W], reads=[('ot', dv)])


NA_KBL = {0: list(range(0, 8)) + [30, 31], 1: list(range(2, 10)), 2: list(range(6, 14)), 3: list(range(8, 16)) + [16, 17]}


def stage_na(C, R, NQ, NK, NV, nab, CAT, scale):
    P = C.P
    with stage(C):
        Kt = P.sb("nK", [128, NTOK], BF16)
        Vt = P.sb("nV", [128, 34, 128], BF16)
        Qt = P.sb("nQ", [128, NOWN], BF16)
        Bt = [P.sb(f"nB{i}", [128, 512], F32) for i in range(3)]
        St = [P.sb(f"nS{i}", [128, 512], F32) for i in range(2)]
        Pt = [P.sb(f"nP{i}", [128, 512], BF16) for i in range(3)]
        rc = P.sb("nrc", [128, 512], F32)
        ot = P.sb("no", [128, 512], BF16)
        cnt = 0
        for h in range(16):
            P.dma('sp', Kt[:, :], NK[h * 128:(h + 1) * 128, :], writes=['K'])
            P.dma('sp', Vt[:, :, :], NV[:, h * 128:(h + 1) * 128].rearrange("(k p) d -> p k d", p=128), writes=['V'])
            P.dma('sp', Qt[:, :], NQ[h * 128:(h + 1) * 128, :], writes=['Q'])
            bi = 0
            for ti in range(5):
                W = 512 if ti < 4 else 128
                o0 = ti * 512 if ti < 4 else 2048
                keys = ([(kb, True) for kb in NA_KBL[ti]] if ti < 4 else []) + [(32, False), (33, False)]
                for pi, (kb, hasb) in enumerate(keys):
                    sb_i = pi % 2
                    pn = cnt % 3
                    cnt += 1
                    P.op('pe', lambda e, i, sb_i=sb_i, kb=kb, o0=o0, W=W: e.matmul(
                        C.banks[sb_i][:, :W], lhsT=Kt[:, kb * 128:(kb + 1) * 128], rhs=Qt[:, o0:o0 + W], start=True, stop=True),
                        reads=['K', 'Q'], writes=[('bank', sb_i)])
                    if hasb:
                        P.dma('act', Bt[pn][:, :], nab[h, bi], writes=[('B', pn)])
                        bi += 1
                        P.op('dve', lambda e, i, sb_i=sb_i, pn=pn, W=W: e.scalar_tensor_tensor(
                            out=St[sb_i][:, :W], in0=C.banks[sb_i][:, :W], scalar=float(scale), in1=Bt[pn][:, :W], op0=ALU.mult, op1=ALU.add),
                            reads=[('bank', sb_i), ('B', pn)], writes=[('S', sb_i)])
                        P.op('act', lambda e, i, sb_i=sb_i, pn=pn, W=W: e.activation(out=Pt[pn][:, :W], in_=St[sb_i][:, :W], func=AF.Exp),
                             reads=[('S', sb_i)], writes=[('P', pn)])
                    else:
                        P.op('act', lambda e, i, sb_i=sb_i, pn=pn, W=W: e.activation(out=Pt[pn][:, :W], in_=C.banks[sb_i][:, :W], func=AF.Exp, scale=float(scale)),
                             reads=[('bank', sb_i)], writes=[('P', pn)])
                    last = pi == len(keys) - 1
                    P.op('pe', lambda e, i, kb=kb, pn=pn, pi=pi, W=W, last=last: e.matmul(
                        C.banks[2][:, :W], lhsT=Vt[:, kb, :], rhs=Pt[pn][:, :W], start=(pi == 0), stop=last),
                        reads=['V', ('P', pn)], writes=[('bank', 2)])
                    P.op('pe', lambda e, i, pn=pn, pi=pi, W=W, last=last: e.matmul(
                        C.banks[3][:, :W], lhsT=R['onesb'][:, :], rhs=Pt[pn][:, :W], start=(pi == 0), stop=last),
                        reads=['onesb', ('P', pn)], writes=[('bank', 3)])
                P.op('dve', lambda e, i, W=W: e.reciprocal(out=rc[:, :W], in_=C.banks[3][:, :W]), reads=[('bank', 3)], writes=['rc'])
                P.op('dve', lambda e, i, W=W: e.tensor_tensor(out=ot[:, :W], in0=C.banks[2][:, :W], in1=rc[:, :W], op=ALU.mult),
                     reads=[('bank', 2), 'rc'], writes=['ot'])
                r0 = 2048 + h * 128
                P.dma('sp', CAT[r0:r0 + 128, o0:o0 + W], ot[:, :W], reads=['ot'])


def stage_lin_out(C, src, K, blocks, Ws, ncols, out, tag, tb=1024, epi=None, ntile=4):
    P = C.P
    with stage(C):
        NB = 4
        odt = BF16 if epi == 'swiglu' else F32
        ob = [P.sb(f"lo_{tag}{i}", [128, 512], odt) for i in range(NB)]
        tm = [P.sb(f"lt_{tag}{i}", [128, 512], F32) for i in range(2)]
        cnt = [0]

        def evac(j, si, c, t0, s, sl, bl):
            n = cnt[0] % NB
            m = cnt[0] % 2
            cnt[0] += 1
            if epi == 'swiglu':
                bg, bu = bl
                P.op('act', lambda e, i: e.activation(out=tm[m][:, :sl], in_=C.banks[bg][:, :sl], func=AF.Silu), reads=[('bank', bg)], writes=[('tm', m)])
                P.op('dve', lambda e, i: e.tensor_tensor(out=ob[n][:, :sl], in0=tm[m][:, :sl], in1=C.banks[bu][:, :sl], op=ALU.mult),
                     reads=[('tm', m), ('bank', bu)], writes=[('ob', n)])
            else:
                copy_op(C, C.alt(), ob[n][:, :sl], C.banks[bl[0]][:, :sl], [('bank', bl[0])], [('ob', n)])
            P.dma('sp', out[c:c + 128, t0 + s:t0 + s + sl], ob[n][:, :sl], reads=[('ob', n)])

        lin(C, None, src, K, blocks, Ws, ncols, 'fm', evac, tag, ntile=ntile, tb=tb)


def own_x_col(o0):
    return o0 if o0 < 2048 else SEQ + (o0 - 2048)


OWN256 = [(i * 256, 256, 0) for i in range(8)] + [(2048, 128, 1)]


def stage_resid_norm(C, R, xT, OT, XM, H2, blocks=None, ident=False, HF=None):
    P = C.P
    with stage(C):
        xs = [P.sb(f"zx{i}", [128, KC, 256], F32) for i in range(2)]
        blocks = OWN256 if blocks is None else blocks
        os_ = [P.sb(f"zo{i}", [128, KC, 256], F32) for i in range(1 if HF is not None else 2)]
        hs = P.sb("zh", [128, KC, 256], BF16)
        hf32 = P.sb("zhf", [128, KC, 256], F32) if HF is not None else None
        hfv = HF.rearrange("(c p) t -> p c t", p=128) if HF is not None else None
        xv = xT.rearrange("(c p) t -> p c t", p=128)
        ov = OT.rearrange("(c p) t -> p c t", p=128)
        xmv = XM.rearrange("(c p) t -> p c t", p=128)
        hv = H2.rearrange("(c p) t -> p c t", p=128)
        for bi, (o0, tl, k) in enumerate(blocks):
            ob_i = bi % len(os_)
            xb, obf = xs[bi % 2], os_[ob_i]
            xc0 = o0 if ident else own_x_col(o0)
            for q in range(4):
                P.dma('sp', xb[:, q * 8:(q + 1) * 8, :tl], xv[:, q * 8:(q + 1) * 8, xc0:xc0 + tl], writes=[('x', bi % 2, q)])
                P.dma('act', obf[:, q * 8:(q + 1) * 8, :tl], ov[:, q * 8:(q + 1) * 8, o0:o0 + tl], writes=[('o', ob_i, q)])
            rms_bcast(C, R, obf, tl, KC, 'rb', D, lambda kc, ob_i=ob_i: ('o', ob_i, kc // 8))
            for kc in range(KC):
                t = R['tmp'][kc % 3]
                P.op('dve', lambda e, i, t=t, kc=kc, obf=obf, tl=tl: e.tensor_tensor(out=t[:, :tl], in0=obf[:, kc, :tl], in1=R['rb'][:, :tl], op=ALU.mult),
                     reads=[('o', ob_i, kc // 8), 'rb'], writes=[('tmp', kc % 3)])
                P.op('dve', lambda e, i, t=t, kc=kc, xb=xb, tl=tl, k=k: e.scalar_tensor_tensor(
                    out=xb[:, kc, :tl], in0=t[:, :tl], scalar=R['A'][:, kc, 2 + k:3 + k], in1=xb[:, kc, :tl], op0=ALU.mult, op1=ALU.add),
                    reads=[('tmp', kc % 3), ('x', bi % 2, kc // 8)], writes=[('x', bi % 2, kc // 8)])
            for q in range(4):
                P.dma('sp', xmv[:, q * 8:(q + 1) * 8, o0:o0 + tl], xb[:, q * 8:(q + 1) * 8, :tl], reads=[('x', bi % 2, q)])
            rms_bcast(C, R, xb, tl, KC, 'rb', D, lambda kc, bi=bi: ('x', bi % 2, kc // 8))
            for kc in range(KC):
                t = R['tmp'][kc % 3]
                P.op('dve', lambda e, i, t=t, kc=kc, xb=xb, tl=tl: e.tensor_tensor(out=t[:, :tl], in0=xb[:, kc, :tl], in1=R['rb'][:, :tl], op=ALU.mult),
                     reads=[('x', bi % 2, kc // 8), 'rb'], writes=[('tmp', kc % 3)])
                P.op('act', lambda e, i, t=t, kc=kc, k=k, tl=tl: e.activation(out=hs[:, kc, :tl], in_=t[:, :tl], func=AF.Identity,
                                                                             scale=R['A'][:, kc, 4 + k:5 + k], bias=R['vec'][:, kc, 6 * k + 4:6 * k + 5]),
                     reads=[('tmp', kc % 3)], writes=[('h', kc // 8)])
                if HF is not None:
                    P.op('act', lambda e, i, t=t, kc=kc, k=k, tl=tl: e.activation(out=hf32[:, kc, :tl], in_=t[:, :tl], func=AF.Identity,
                                                                                 scale=R['A'][:, kc, 4 + k:5 + k], bias=R['vec'][:, kc, 6 * k + 4:6 * k + 5]),
                         reads=[('tmp', kc % 3)], writes=[('hf', kc // 8)])
            for q in range(4):
                P.dma('pool', hv[:, q * 8:(q + 1) * 8, o0:o0 + tl], hs[:, q * 8:(q + 1) * 8, :tl], reads=[('h', q)])
                if HF is not None:
                    P.dma('sp', hfv[:, q * 8:(q + 1) * 8, o0:o0 + tl], hf32[:, q * 8:(q + 1) * 8, :tl], reads=[('hf', q)])


def stage_final(C, R, XM, YT, out_l, out_c, blocks=None):
    P = C.P
    with stage(C):
        xs = [P.sb(f"fx{i}", [128, KC, 256], F32) for i in range(2)]
        ys = [P.sb(f"fy{i}", [128, KC, 256], F32) for i in range(2)]
        xmv = XM.rearrange("(c p) t -> p c t", p=128)
        yv = YT.rearrange("(c p) t -> p c t", p=128)
        olv = out_l.rearrange("(c p) t -> p c t", p=128)
        ocv = out_c.rearrange("(c p) t -> p c t", p=128) if out_c is not None else None
        for bi, (o0, tl, k) in enumerate(OWN256 if blocks is None else blocks):
            if k == 1 and out_c is None:
                continue
            xb, yb = xs[bi % 2], ys[bi % 2]
            for q in range(4):
                P.dma('sp', xb[:, q * 8:(q + 1) * 8, :tl], xmv[:, q * 8:(q + 1) * 8, o0:o0 + tl], writes=[('x', bi % 2, q)])
                P.dma('act', yb[:, q * 8:(q + 1) * 8, :tl], yv[:, q * 8:(q + 1) * 8, o0:o0 + tl], writes=[('y', bi % 2, q)])
            rms_bcast(C, R, yb, tl, KC, 'rb', D, lambda kc, bi=bi: ('y', bi % 2, kc // 8))
            for kc in range(KC):
                t = R['tmp'][kc % 3]
                P.op('dve', lambda e, i, t=t, kc=kc, yb=yb, tl=tl: e.tensor_tensor(out=t[:, :tl], in0=yb[:, kc, :tl], in1=R['rb'][:, :tl], op=ALU.mult),
                     reads=[('y', bi % 2, kc // 8), 'rb'], writes=[('tmp', kc % 3)])
                P.op('dve', lambda e, i, t=t, kc=kc, xb=xb, tl=tl, k=k: e.scalar_tensor_tensor(
                    out=xb[:, kc, :tl], in0=t[:, :tl], scalar=R['A'][:, kc, 6 + k:7 + k], in1=xb[:, kc, :tl], op0=ALU.mult, op1=ALU.add),
                    reads=[('tmp', kc % 3), ('x', bi % 2, kc // 8)], writes=[('x', bi % 2, kc // 8)])
            for q in range(4):
                dst = olv[:, q * 8:(q + 1) * 8, o0:o0 + tl] if k == 0 else ocv[:, q * 8:(q + 1) * 8, 0:tl]
                P.dma('sp', dst, xb[:, q * 8:(q + 1) * 8, :tl], reads=[('x', bi % 2, q)])


NUNITS = 2
NCORES = 8 // NUNITS


def build_layer0():
    nc = _new_nc()
    di = lambda name, shape, dt=F32: nc.dram_tensor(name, list(shape), dt, kind="ExternalInput").ap()
    w_in = di("w_in", [D, 14336])
    w_out = di("w_out", [D, D])
    wg = di("wg", [D, 11008])
    wu = di("wu", [D, 11008])
    wd = di("wd", [11008, D])
    U = []
    for u in range(NUNITS):
        U.append(dict(
            xT=di(f"xT_{u}", [D, NTOK]), vec=di(f"vec_{u}", [128, KC, 16]), rdec=di(f"rdec_{u}", [128, 16]),
            etab=di(f"etab_{u}", [128, 2304]), nexp=di(f"nexp_{u}", [128, 2, 170]), cmask=di(f"cmask_{u}", [128, 8, 512]),
            ropetab=di(f"ropetab_{u}", [2, 2, 128, SEQ]), nab=di(f"nab_{u}", [16, 36, 128, 512]),
            out_l=nc.dram_tensor(f"out_l_{u}", [D, SEQ // 2], F32, kind="ExternalOutput").ap(),
            out_c=nc.dram_tensor(f"out_c_{u}", [D, LCTX // 2], F32, kind="ExternalOutput").ap()))
    with ExitStack() as st:
        C = Ctx(nc, st)
        hT = C.scratch("hT", [D, NTOK], BF16)
        QF = C.scratch("QF", [2048, NOWN], BF16)
        QB = C.scratch("QB", [2048, NOWN], BF16)
        KF = C.scratch("KF", [2048, NTOK], BF16)
        KB = C.scratch("KB", [2048, NTOK], BF16)
        RV = C.scratch("RV", [NTOK, 2048], BF16)
        RG = C.scratch("RG", [2048, NOWN], BF16)
        NQ = C.scratch("NQ", [2048, NOWN], BF16)
        NK = C.scratch("NK", [2048, NTOK], BF16)
        NV = C.scratch("NV", [NTOK, 2048], BF16)
        CAT = C.scratch("CAT", [D, NOWN], BF16)
        OT = C.scratch("OT", [D, NOWN], F32)
        XM = C.scratch("XM", [D, NOWN], F32)
        H2 = C.scratch("H2", [D, NOWN], BF16)
        FH = C.scratch("FH", [11008, NOWN], BF16)
        YT = C.scratch("YT", [D, NOWN], F32)
        for u in range(NUNITS):
            I = U[u]
            with ExitStack() as ust:
                old = C.P.stack
                C.P.stack = ust
                R = load_consts(C, I['vec'])
                compute_loggamma(C, R, I['rdec'])
                nblocks = [(i * 512, 512, 0) for i in range(8)] + [(4096, 256, 1)]
                stage_norm_in(C, R, I['xT'], hT, nblocks)
                ident = lambda t0: t0
                opos = lambda t0: t0 if t0 < 2048 else 2048
                tg = f"{u}"
                inproj_rope(C, R, hT, w_in, 0, OWN_BLOCKS_H, opos, I['etab'], I['ropetab'], QF, QB, True, "rq" + tg)
                inproj_rope(C, R, hT, w_in, 2048, ALL_BLOCKS, ident, I['etab'], I['ropetab'], KF, KB, False, "rk" + tg)
                inproj_plain(C, R, hT, w_in, 4096, ALL_BLOCKS, ident, RV, 'tm', "rv" + tg)
                inproj_plain(C, R, hT, w_in, 6144, OWN_BLOCKS_H, opos, RG, 'fm', "rg" + tg, func=AF.Silu)
                inproj_plain(C, R, hT, w_in, 8192, OWN_BLOCKS_H, opos, NQ, 'fm', "nq" + tg)
                inproj_plain(C, R, hT, w_in, 10240, ALL_BLOCKS, ident, NK, 'fm', "nk" + tg)
                inproj_plain(C, R, hT, w_in, 12288, ALL_BLOCKS, ident, NV, 'tm', "nv" + tg)
                stage_retention(C, R, QF, QB, KF, KB, RV, RG, CAT, I['nexp'], I['cmask'])
                stage_na(C, R, NQ, NK, NV, I['nab'], CAT, 128 ** -0.5)
                stage_lin_out(C, CAT, D, OWN_BLOCKS_O, [w_out], D, OT, "wo" + tg)
                stage_resid_norm(C, R, I['xT'], OT, XM, H2)
                stage_lin_out(C, H2, D, OWN_BLOCKS_O, [wg, wu], 11008, FH, "gu" + tg, epi='swiglu', ntile=2)
                blocks512 = [(0, 512), (512, 512), (1024, 512), (1536, 512), (2048, 128)]
                stage_lin_out(C, FH, 11008, blocks512, [wd], D, YT, "wd" + tg, tb=512)
                stage_final(C, R, XM, YT, I['out_l'], I['out_c'])
                C.P.flush()
                C.P.stack = old
        C.P.flush(final=True)
        print("layer0 instructions:", C.P.n_instr)
    return nc


def _pvec(v):
    return np.ascontiguousarray(np.asarray(v, np.float32).reshape(KC, 128).T)


def l0_constants(s):
    dirs = (1, 0) if s == 0 else (0, 1)
    il = np.arange(512, dtype=np.float32)
    ilc = np.arange(128, dtype=np.float32)
    jj = (np.arange(512) % 128).astype(np.float32)
    etab = np.zeros(2304, np.float32)
    for d, dr in enumerate(dirs):
        etab[d * 512:(d + 1) * 512] = il if dr == 0 else 512 - il
        etab[1024 + d * 128:1024 + (d + 1) * 128] = ilc if dr == 0 else 128 - ilc
        etab[1280 + d * 512:1280 + (d + 1) * 512] = (128 - jj) if dr == 0 else jj
    BIG = 1e9
    nexp = np.full((2, 5, 34), BIG, np.float32)
    for d, dr in enumerate(dirs):
        for ti in range(5):
            for kb in range(34):
                if kb < 16:
                    t0, isc = 2048 * s + 128 * kb, False
                elif kb < 32:
                    t0, isc = 2048 * (1 - s) + 128 * (kb - 16), False
                else:
                    t0, isc = (128 * s if kb == 32 else 128 * (1 - s)), True
                if ti < 4:
                    bq = 2048 * s + 512 * ti
                    if not isc:
                        if dr == 0 and t0 <= bq + 511:
                            nexp[d, ti, kb] = bq - t0 - 128
                        if dr == 1 and t0 + 127 >= bq:
                            nexp[d, ti, kb] = t0 - bq - 512
                    else:
                        nexp[d, ti, kb] = (128 + bq - t0) if dr == 0 else (3584 - bq + t0)
                else:
                    if isc:
                        if dr == 0 and t0 <= 128 * s:
                            nexp[d, ti, kb] = 128 * s - t0 - 128
                        if dr == 1 and t0 >= 128 * s:
                            nexp[d, ti, kb] = t0 - 128 * s - 128
    cm = np.zeros((8, 128, 512), np.float32)
    jv = np.arange(128)[:, None]
    iv = np.arange(512)[None, :]
    for d, dr in enumerate(dirs):
        for r in range(4):
            cm[d * 4 + r] = ((128 * r + jv <= iv) if dr == 0 else (128 * r + jv >= iv)).astype(np.float32)
    p = np.arange(SEQ)
    tact = np.where(p < 2048, 2048 * s + p, 2048 * (1 - s) + (p - 2048))
    row = (tact // 64).astype(np.float32)
    col = (tact % 64).astype(np.float32)
    inv = (10000.0 ** (-np.arange(64, dtype=np.float32) / 64)).astype(np.float32)
    f = np.arange(128) % 64
    sign = np.where(np.arange(128) < 64, -1.0, 1.0).astype(np.float32)[:, None]
    rt = np.zeros((2, 2, 128, SEQ), np.float32)
    for ct, pos in enumerate((row, col)):
        ang = (pos[None, :] * inv[f][:, None]).astype(np.float32)
        rt[ct, 0] = np.cos(ang)
        rt[ct, 1] = sign * np.sin(ang)
    na_idx = []
    for ti in range(4):
        ilv = np.arange(512)
        r_act = 32 * s + 8 * ti + ilv // 64
        cq = ilv % 64
        r0 = np.clip(r_act - 4, 0, 56)
        cst = np.clip(cq - 8, 0, 48)
        for kb in NA_KBL[ti]:
            jjv = np.arange(128)
            rk = (32 * s + 2 * kb if kb < 16 else 32 * (1 - s) + 2 * (kb - 16)) + jjv // 64
            ck = jjv % 64
            ok = (rk[:, None] >= r0[None, :]) & (rk[:, None] < r0[None, :] + 8) & (ck[:, None] >= cst[None, :]) & (ck[:, None] < cst[None, :] + 16)
            dr_i = np.clip(rk[:, None] - r_act[None, :] + 7, 0, 14)
            dc_i = np.clip(ck[:, None] - cq[None, :] + 15, 0, 30)
            na_idx.append((ok, dr_i, dc_i))
    rep = lambda a: np.ascontiguousarray(np.broadcast_to(a.reshape(1, -1), (128, a.size)))
    return dict(dirs=dirs, etab=rep(etab), nexp=rep(nexp.reshape(-1)).reshape(128, 2, 170),
                cmask=np.ascontiguousarray(cm.transpose(1, 0, 2)), ropetab=rt, na_idx=na_idx)


def l0_inputs(inp, mod, b, s, consts):
    x, ctx = inp['x'], inp['ctx']
    o = 1 - s
    xt = np.concatenate([x[b, 2048 * s:2048 * (s + 1)], x[b, 2048 * o:2048 * (o + 1)],
                         ctx[b, 128 * s:128 * (s + 1)], ctx[b, 128 * o:128 * (o + 1)]], axis=0)
    vec = np.zeros((128, KC, 16), np.float32)
    for j in range(6):
        vec[:, :, j] = _pvec(mod[0, b, j])
        vec[:, :, 6 + j] = _pvec(mod[0, 4, j])
    for j in range(4):
        vec[:, :, 12 + j] = _pvec(inp['norm_g'][0, j])
    rd = np.asarray(inp['ret_decay'], np.float32)[0]
    rdec = np.concatenate([rd[consts['dirs'][0]], rd[consts['dirs'][1]]])
    rpb = np.asarray(inp['na_rpb'], np.float32)[0]
    nab = np.empty((16, 36, 128, 512), np.float32)
    for k, (ok, dr_i, dc_i) in enumerate(consts['na_idx']):
        nab[:, k] = np.where(ok[None], rpb[:, dr_i, dc_i], np.float32(-1e30))
    return {
        "xT": np.ascontiguousarray(xt.T), "vec": vec,
        "rdec": np.ascontiguousarray(np.broadcast_to(rdec[None], (128, 16))),
        "etab": consts['etab'], "nexp": consts['nexp'], "cmask": consts['cmask'], "ropetab": consts['ropetab'], "nab": nab,
    }


TIME_BLOCKS = [(0, 1024), (1024, 1024), (2048, 1024), (3072, 1024), (4096, 256)]
OWNL_BLOCKS = [(0, 1024), (1024, 1024)]
NL = SEQ // 2


def stage_norm_simple(C, R, xT, hT, blocks, hF=None):
    stage_norm_in(C, R, xT, hT, blocks)


def l1_inproj(C, R, hT, W, c0, ncols, blocks, handlers, tag):
    P = C.P
    with stage(C):
        def evac(j, si, c, t0, s, sl, bl):
            for lo, hi, fn in handlers:
                if lo <= c < hi:
                    fn(c - lo, t0, s, sl, bl[0])
                    return
            raise AssertionError(c)
        lin(C, None, hT, D, blocks, [W[:, c0:c0 + ncols]], ncols, 'fm', evac, tag)


class Ring:
    def __init__(self, P, name, shape, dt, n):
        self.t = [P.sb(f"{name}{i}", shape, dt) for i in range(n)]
        self.name = name
        self.i = 0

    def next(self):
        k = self.i % len(self.t)
        self.i += 1
        return self.t[k], (self.name, k)


def rope_small(C, src, srck, n, swp, cs_ap, pos, sl, nblk):
    P = C.P
    sw, swk = swp
    for blk in range(nblk):
        for q in range(4):
            a = blk * 64 + q * 16
            b = blk * 64 + (q ^ 1) * 16
            P.dma('act', sw[a:a + 16, :sl], src[b:b + 16, :sl], reads=[srck], writes=[(swk, blk, q)])
    return [(swk, blk, q) for blk in range(nblk) for q in range(4)]


def build_layer1():
    nc = _new_nc()
    di = lambda name, shape, dt=F32: nc.dram_tensor(name, list(shape), dt, kind="ExternalInput").ap()
    xT = di("xT", [D, NTOK])
    vec = di("vec", [128, KC, 16])
    w_in = di("w_in", [D, 6272])
    gq = di("gq", [128, 12])
    wuq = di("wuq", [1536, 3072])
    gkv = di("gkv", [128, 4])
    wukv = di("wukv", [512, 4096])
    cw = di("cw", [128, 16, 4])
    wa = di("wa", [2, 16, 128, 128])
    wi = di("wi", [2, 16, 128, 128])
    w_out = di("w_out", [D, D])
    routerB = di("routerB", [8, D, 128])
    mwg = di("mwg", [8, D, D])
    mwu = di("mwu", [8, D, D])
    mwd = di("mwd", [8, D, D])
    ropeK = di("ropeK", [2, 128, SEQ])
    UU = []
    for u in range(NUNITS):
        UU.append(dict(xO=di(f"xO_{u}", [D, NL]), lvec=di(f"lvec_{u}", [128, 16, 8]), ropeQ=di(f"ropeQ_{u}", [2, 128, NL]),
                       out_l=nc.dram_tensor(f"out_l_{u}", [D, NL], F32, kind="ExternalOutput").ap()))
    with ExitStack() as st:
        C = Ctx(nc, st)
        P = C.P
        sc = C.scratch
        hT, hO = sc("hT", [D, NTOK], BF16), sc("hO", [D, NL], BF16)
        CKV, LX, CQ = sc("CKV", [512, NTOK], F32), sc("LX", [2048, NTOK], F32), sc("CQ", [1536, NL], F32)
        KPE, LY = sc("KPE", [128, NTOK], BF16), sc("LY", [2048, NL], BF16)
        CQN, CKVN = sc("CQN", [1536, NL], BF16), sc("CKVN", [512, NTOK], BF16)
        QN, QPE = sc("QN", [2048, NL], BF16), sc("QPE", [1024, NL], BF16)
        KN, VV = sc("KN", [2048, NTOK], BF16), sc("VV", [NTOK, 2048], BF16)
        CAT, OT = sc("CAT", [D, NL], BF16), sc("OT", [D, NL], F32)
        XM, H2, H2F = sc("XM", [D, NL], F32), sc("H2", [D, NL], BF16), sc("H2F", [D, NL], F32)
        COMB = sc("COMB", [8, 128, NL], F32)
        FH, YT = sc("FH", [8 * D, NL], BF16), sc("YT", [D, NL], F32)
        for u in range(NUNITS):
            l1_unit(C, u, locals())
        C.P.flush(final=True)
        print("layer1 instructions:", C.P.n_instr)
    return nc


def l1_unit(C, u, G):
    P = C.P
    g = lambda k: G[k]
    xT, vec, w_in, gq, wuq, gkv, wukv, cw, wa, wi, w_out, routerB, mwg, mwu, mwd, ropeK = [G[k] for k in (
        "xT", "vec", "w_in", "gq", "wuq", "gkv", "wukv", "cw", "wa", "wi", "w_out", "routerB", "mwg", "mwu", "mwd", "ropeK")]
    hT, hO, CKV, LX, CQ, KPE, LY, CQN, CKVN, QN, QPE, KN, VV, CAT, OT, XM, H2, H2F, COMB, FH, YT = [G[k] for k in (
        "hT", "hO", "CKV", "LX", "CQ", "KPE", "LY", "CQN", "CKVN", "QN", "QPE", "KN", "VV", "CAT", "OT", "XM", "H2", "H2F", "COMB", "FH", "YT")]
    I = G["UU"][u]
    xO, lvec, ropeQ, out_l = I['xO'], I['lvec'], I['ropeQ'], I['out_l']
    tg = f"{u}"
    with ExitStack() as ust:
        old_stack = C.P.stack
        C.P.stack = ust
        R = load_consts(C, vec)
        stage_norm_in(C, R, xT, hT, [(i * 512, 512, 0) for i in range(8)] + [(4096, 256, 1)])
        stage_norm_in(C, R, xO, hO, [(i * 512, 512, 0) for i in range(4)])

        with stage(C):
            o32 = Ring(P, "i32_", [128, 512], F32, 4)
            sw = Ring(P, "isw_", [128, 512], F32, 2)
            cs = Ring(P, "ics_", [128, 2, 512], F32, 2)
            o16 = Ring(P, "i16_", [128, 512], BF16, 2)

            def ev_all(j, si, c, t0, s, sl, bl):
                b = bl[0]
                pos = t0 + s
                t, tk = o32.next()
                copy_op(C, C.alt(), t[:, :sl], C.banks[b][:, :sl], [('bank', b)], [tk])
                if c < 512:
                    P.dma('sp', CKV[c:c + 128, pos:pos + sl], t[:, :sl], reads=[tk])
                elif c < 640:
                    o, ok = o16.next()
                    if pos < SEQ:
                        w_, wk = sw.next()
                        c_, ck = cs.next()
                        keys = rope_small(C, t, tk, 0, (w_, wk), None, pos, sl, 2)
                        P.dma('sp', c_[:, :, :sl], ropeK[:, :, pos:pos + sl].rearrange("k p t -> p k t"), writes=[ck])
                        P.op('dve', lambda e, i: e.tensor_tensor(out=t[:, :sl], in0=t[:, :sl], in1=c_[:, 0, :sl], op=ALU.mult), reads=[tk, ck], writes=[tk])
                        P.op('dve', lambda e, i: e.tensor_tensor(out=w_[:, :sl], in0=w_[:, :sl], in1=c_[:, 1, :sl], op=ALU.mult), reads=keys + [ck], writes=keys)
                        P.op('dve', lambda e, i: e.tensor_tensor(out=o[:, :sl], in0=t[:, :sl], in1=w_[:, :sl], op=ALU.add), reads=keys + [tk], writes=[ok])
                    else:
                        P.op('dve', lambda e, i: e.tensor_copy(out=o[:, :sl], in_=t[:, :sl]), reads=[tk], writes=[ok])
                    P.dma('sp', KPE[:, pos:pos + sl], o[:, :sl], reads=[ok])
                else:
                    r0 = c - 640
                    P.dma('sp', LX[r0:r0 + 128, pos:pos + sl], t[:, :sl], reads=[tk])
            lin(C, None, hT, D, TIME_BLOCKS, [w_in[:, 1536:1536 + 2688]], 2688, 'fm', ev_all, "ia" + tg)

        with stage(C):
            o32 = Ring(P, "j32_", [128, 512], F32, 4)
            g1 = Ring(P, "jg1_", [128, 512], F32, 2)
            o16 = Ring(P, "j16_", [128, 512], BF16, 2)

            def ev_own(j, si, c, t0, s, sl, bl):
                b = bl[0]
                pos = t0 + s
                t, tk = o32.next()
                copy_op(C, 'act', t[:, :sl], C.banks[b][:, :sl], [('bank', b)], [tk])
                if c < 1536:
                    P.dma('sp', CQ[c:c + 128, pos:pos + sl], t[:, :sl], reads=[tk])
                else:
                    u, uk = g1.next()
                    o, ok = o16.next()
                    P.op('act', lambda e, i: e.activation(out=u[:, :sl], in_=t[:, :sl], func=AF.Square), reads=[tk], writes=[uk])
                    P.op('dve', lambda e, i: e.tensor_scalar(out=u[:, :sl], in0=u[:, :sl], scalar1=0.044715, scalar2=1.0, op0=ALU.mult, op1=ALU.add), reads=[uk], writes=[uk])
                    P.op('dve', lambda e, i: e.tensor_tensor(out=u[:, :sl], in0=u[:, :sl], in1=t[:, :sl], op=ALU.mult), reads=[uk, tk], writes=[uk])
                    P.op('act', lambda e, i: e.activation(out=u[:, :sl], in_=u[:, :sl], func=AF.Sigmoid, scale=1.5957691216057308), reads=[uk], writes=[uk])
                    P.op('dve', lambda e, i: e.tensor_tensor(out=o[:, :sl], in0=u[:, :sl], in1=t[:, :sl], op=ALU.mult), reads=[uk, tk], writes=[ok])
                    r0 = c - 1536
                    P.dma('sp', LY[r0:r0 + 128, pos:pos + sl], o[:, :sl], reads=[ok])
            wsel = lambda c: c
            lin(C, None, hO, D, OWNL_BLOCKS, [w_in[:, 0:1536]], 1536, 'fm', ev_own, "io1" + tg)
        with stage(C):
            o32 = Ring(P, "k32_", [128, 512], F32, 4)
            g1 = Ring(P, "kg1_", [128, 512], F32, 2)
            o16 = Ring(P, "k16_", [128, 512], BF16, 2)
            lin(C, None, hO, D, OWNL_BLOCKS, [w_in[:, 4224:6272]], 2048, 'fm',
                lambda j, si, c, t0, s, sl, bl: ev_own(j, si, c + 1536, t0, s, sl, bl), "io2" + tg)

        def small_norm(src, dst, nch, T, gv_ap, tagn):
            with stage(C):
                g_sb = P.sb(f"sg_{tagn}", [128, nch], F32)
                P.dma('sp', g_sb[:, :], gv_ap[:, :], writes=['g'])
                xs_ = [P.sb(f"sx_{tagn}{i}", [128, nch, 512], F32) for i in range(2)]
                hs_ = P.sb(f"sh_{tagn}", [128, nch, 512], BF16)
                sv = src.rearrange("(c p) t -> p c t", p=128)
                dv_ = dst.rearrange("(c p) t -> p c t", p=128)
                for bi, t0 in enumerate(range(0, T, 512)):
                    tl = min(512, T - t0)
                    xb = xs_[bi % 2]
                    P.dma('sp', xb[:, :, :tl], sv[:, :, t0:t0 + tl], writes=[('x', bi % 2)])
                    rms_bcast(C, R, xb, tl, nch, 'rb', nch * 128, lambda kc, bi=bi: ('x', bi % 2))
                    for kc in range(nch):
                        t = R['tmp'][kc % 3]
                        P.op('dve', lambda e, i, t=t, kc=kc, xb=xb, tl=tl: e.tensor_tensor(out=t[:, :tl], in0=xb[:, kc, :tl], in1=R['rb'][:, :tl], op=ALU.mult),
                             reads=[('x', bi % 2), 'rb'], writes=[('tmp', kc % 3)])
                        P.op('act', lambda e, i, t=t, kc=kc, tl=tl: e.activation(out=hs_[:, kc, :tl], in_=t[:, :tl], func=AF.Identity, scale=g_sb[:, kc:kc + 1]),
                             reads=[('tmp', kc % 3), 'g'], writes=['h'])
                    P.dma('pool', dv_[:, :, t0:t0 + tl], hs_[:, :, :tl], reads=['h'])
        small_norm(CQ, CQN, 12, NL, gq, "q")
        small_norm(CKV, CKVN, 4, NTOK, gkv, "kv")

        with stage(C):
            o32 = Ring(P, "q32_", [128, 512], F32, 3)
            sw = Ring(P, "qsw_", [128, 512], F32, 2)
            cs = Ring(P, "qcs_", [128, 2, 512], F32, 2)
            o16 = Ring(P, "q16_", [128, 512], BF16, 3)

            def ev_q(j, si, c, t0, s, sl, bl):
                b = bl[0]
                pos = t0 + s
                o, ok = o16.next()
                if c < 2048:
                    copy_op(C, C.alt(), o[:, :sl], C.banks[b][:, :sl], [('bank', b)], [ok])
                    P.dma('sp', QN[c:c + 128, pos:pos + sl], o[:, :sl], reads=[ok])
                else:
                    t, tk = o32.next()
                    w_, wk = sw.next()
                    c_, ck = cs.next()
                    copy_op(C, 'act', t[:, :sl], C.banks[b][:, :sl], [('bank', b)], [tk])
                    keys = rope_small(C, t, tk, 0, (w_, wk), None, pos, sl, 2)
                    P.dma('sp', c_[:, :, :sl], ropeQ[:, :, pos:pos + sl].rearrange("k p t -> p k t"), writes=[ck])
                    P.op('dve', lambda e, i: e.tensor_tensor(out=t[:, :sl], in0=t[:, :sl], in1=c_[:, 0, :sl], op=ALU.mult), reads=[tk, ck], writes=[tk])
                    P.op('dve', lambda e, i: e.tensor_tensor(out=w_[:, :sl], in0=w_[:, :sl], in1=c_[:, 1, :sl], op=ALU.mult), reads=keys + [ck], writes=keys)
                    P.op('dve', lambda e, i: e.tensor_tensor(out=o[:, :sl], in0=t[:, :sl], in1=w_[:, :sl], op=ALU.add), reads=keys + [tk], writes=[ok])
                    r0 = c - 2048
                    P.dma('sp', QPE[r0:r0 + 128, pos:pos + sl], o[:, :sl], reads=[ok])
            lin(C, None, CQN, 1536, OWNL_BLOCKS, [wuq], 3072, 'fm', ev_q, "uq" + tg)

        with stage(C):
            o16 = Ring(P, "v16_", [128, 512], BF16, 4)

            def ev_kn(j, si, c, t0, s, sl, bl):
                o, ok = o16.next()
                copy_op(C, C.alt(), o[:, :sl], C.banks[bl[0]][:, :sl], [('bank', bl[0])], [ok])
                P.dma('sp', KN[c:c + 128, t0 + s:t0 + s + sl], o[:, :sl], reads=[ok])
            lin(C, None, CKVN, 512, TIME_BLOCKS, [wukv[:, 0:2048]], 2048, 'fm', ev_kn, "ukn" + tg)
        with stage(C):
            o16 = Ring(P, "w16_", [128, 512], BF16, 4)

            def ev_v(mi, c0, t0, ms, ml, b, ncb):
                o, ok = o16.next()
                copy_op(C, C.alt(), o[:ml, :ncb], C.banks[b][:ml, :ncb], [('bank', b)], [ok])
                P.dma('sp', VV[t0 + ms:t0 + ms + ml, c0:c0 + ncb], o[:ml, :ncb], reads=[ok])
            lin(C, None, CKVN, 512, TIME_BLOCKS, [wukv[:, 2048:4096]], 2048, 'tm', ev_v, "uv" + tg)

        scale = float((128 + 64) ** -0.5)
        with stage(C):
            Kt = P.sb("aK", [128, NTOK], BF16)
            Kp = P.sb("aKp", [128, NTOK], BF16)
            Vt = P.sb("aV", [128, 34, 128], BF16)
            Qt = P.sb("aQ", [128, NL], BF16)
            Qp = P.sb("aQp", [128, NL], BF16)
            Pt = Ring(P, "aP", [128, 512], BF16, 3)
            rc = P.sb("arc", [128, 512], F32)
            ot = P.sb("ao", [128, 512], BF16)
            P.dma('sp', Kp[:, :], KPE[:, :], writes=['Kp'])
            for h in range(16):
                hp = h % 2
                P.dma('sp', Kt[:, :], KN[h * 128:(h + 1) * 128, :], writes=['K'])
                P.dma('sp', Vt[:, :, :], VV[:, h * 128:(h + 1) * 128].rearrange("(k p) d -> p k d", p=128), writes=['V'])
                P.dma('sp', Qt[:, :], QN[h * 128:(h + 1) * 128, :], writes=['Q'])
                if hp == 0:
                    P.dma('sp', Qp[:, :], QPE[(h // 2) * 128:(h // 2 + 1) * 128, :], writes=['Qp'])
                for ti in range(4):
                    o0 = ti * 512
                    for kb in range(34):
                        sb_i = kb % 2
                        p, pk = Pt.next()
                        P.op('pe', lambda e, i, sb_i=sb_i, kb=kb, o0=o0: e.matmul(
                            C.banks[sb_i][:, :], lhsT=Kt[:, kb * 128:(kb + 1) * 128], rhs=Qt[:, o0:o0 + 512], start=True, stop=False),
                            reads=['K', 'Q'], writes=[('bank', sb_i)])
                        P.op('pe', lambda e, i, sb_i=sb_i, kb=kb, o0=o0, hp=hp: e.matmul(
                            C.banks[sb_i][:, :], lhsT=Kp[hp * 64:(hp + 1) * 64, kb * 128:(kb + 1) * 128], rhs=Qp[hp * 64:(hp + 1) * 64, o0:o0 + 512],
                            start=False, stop=True), reads=['Kp', 'Qp'], writes=[('bank', sb_i)])
                        P.op('act', lambda e, i, sb_i=sb_i, p=p: e.activation(out=p[:, :], in_=C.banks[sb_i][:, :], func=AF.Exp, scale=scale),
                             reads=[('bank', sb_i)], writes=[pk])
                        P.op('pe', lambda e, i, kb=kb, p=p: e.matmul(C.banks[2][:, :], lhsT=Vt[:, kb, :], rhs=p[:, :], start=(kb == 0), stop=(kb == 33)),
                             reads=['V', pk], writes=[('bank', 2)])
                        P.op('pe', lambda e, i, kb=kb, p=p: e.matmul(C.banks[3][:, :], lhsT=R['onesb'][:, :], rhs=p[:, :], start=(kb == 0), stop=(kb == 33)),
                             reads=['onesb', pk], writes=[('bank', 3)])
                    P.op('dve', lambda e, i: e.reciprocal(out=rc[:, :], in_=C.banks[3][:, :]), reads=[('bank', 3)], writes=['rc'])
                    P.op('dve', lambda e, i: e.tensor_tensor(out=ot[:, :], in0=C.banks[2][:, :], in1=rc[:, :], op=ALU.mult), reads=[('bank', 2), 'rc'], writes=['ot'])
                    P.dma('sp', CAT[h * 128:(h + 1) * 128, o0:o0 + 512], ot[:, :], reads=['ot'])

        with stage(C):
            lv = P.sb("lv", [128, 16, 8], F32)
            cwt = P.sb("lcw", [128, 16, 4], F32)
            clam = P.sb("lcl", [128, 16, 4], F32)
            one1 = P.sb("lone", [128, 1], F32)
            P.dma('sp', lv[:, :, :], lvec[:, :, :], writes=['lv'])
            P.dma('sp', cwt[:, :, :], cw[:, :, :], writes=['cw'])
            P.op('pool', lambda e, i: e.memset(one1[:, :], 1.0), writes=['one1'])
            for d in range(2):
                P.op('act', lambda e, i, d=d: e.activation(out=clam[:, :, d], in_=lv[:, :, 5 + d], func=AF.Exp, scale=-1.0), reads=['lv'], writes=[('cl', d)])
                P.op('dve', lambda e, i, d=d: e.tensor_scalar(out=clam[:, :, d], in0=clam[:, :, d], scalar1=1.0, scalar2=None, op0=ALU.add), reads=[('cl', d)], writes=[('cl', d)])
                P.op('act', lambda e, i, d=d: e.activation(out=clam[:, :, d], in_=clam[:, :, d], func=AF.Ln), reads=[('cl', d)], writes=[('cl', d)])
                P.op('dve', lambda e, i, d=d: e.tensor_scalar(out=clam[:, :, d], in0=clam[:, :, d], scalar1=-8.0, scalar2=None, op0=ALU.mult), reads=[('cl', d)], writes=[('cl', d)])
                P.op('dve', lambda e, i, d=d: e.tensor_scalar(out=clam[:, :, 2 + d], in0=clam[:, :, d], scalar1=2.0, scalar2=None, op0=ALU.mult), reads=[('cl', d)], writes=[('cl2', d)])
            xr = P.sb("lxr", [128, NTOK], F32)
            xc_ = P.sb("lxc", [128, NTOK], F32)
            xb16 = P.sb("lxb", [128, NTOK], BF16)
            av = P.sb("la", [128, NTOK], F32)
            uv = P.sb("lu", [128, NTOK], F32)
            hf = P.sb("lhf", [128, NTOK], F32)
            hb_ = P.sb("lhb", [128, NTOK], F32)
            wat = [P.sb(f"lwa{d}", [128, 128], BF16) for d in range(2)]
            wit = [P.sb(f"lwi{d}", [128, 128], BF16) for d in range(2)]
            lyt = P.sb("lly", [128, NL], BF16)
            oo = P.sb("loo", [128, NL], BF16)
            segs = [(0, SEQ), (SEQ, LCTX)]
            for k in range(16):
                P.dma('sp', xr[:, :], LX[k * 128:(k + 1) * 128, :], writes=['xr'])
                for d in range(2):
                    P.dma('pool', wat[d][:, :], wa[d, k], writes=[('wa', d)])
                    P.dma('pool', wit[d][:, :], wi[d, k], writes=[('wi', d)])
                P.dma('act', lyt[:, :], LY[k * 128:(k + 1) * 128, :], writes=['ly'])
                for (a0, n) in segs:
                    P.op('act', lambda e, i, a0=a0, n=n, k=k: e.activation(out=xc_[:, a0:a0 + n], in_=xr[:, a0:a0 + n], func=AF.Identity,
                                                                          scale=cwt[:, k, 1:2], bias=lv[:, k, 0:1]), reads=['xr', 'cw', 'lv'], writes=['xc'])
                    for (j, so, do, ln_) in ((0, 0, 1, n - 1), (2, 1, 0, n - 1), (3, 2, 0, n - 2)):
                        P.op('dve', lambda e, i, a0=a0, j=j, so=so, do=do, ln_=ln_, k=k: e.scalar_tensor_tensor(
                            out=xc_[:, a0 + do:a0 + do + ln_], in0=xr[:, a0 + so:a0 + so + ln_], scalar=cwt[:, k, j:j + 1], in1=xc_[:, a0 + do:a0 + do + ln_],
                            op0=ALU.mult, op1=ALU.add), reads=['xr', 'xc'], writes=['xc'])
                P.op('act', lambda e, i: e.copy(out=xb16[:, :], in_=xc_[:, :]), reads=['xc'], writes=['xb'])
                for d in range(2):
                    for (s0, sl) in [(q * 512, 512) for q in range(8)] + [(4096, 256)]:
                        P.op('pe', lambda e, i, d=d, s0=s0, sl=sl: e.matmul(C.banks[0][:, :sl], lhsT=wat[d][:, :], rhs=xb16[:, s0:s0 + sl], start=True, stop=True),
                             reads=[('wa', d), 'xb'], writes=[('bank', 0)])
                        P.op('pe', lambda e, i, d=d, s0=s0, sl=sl: e.matmul(C.banks[1][:, :sl], lhsT=wit[d][:, :], rhs=xb16[:, s0:s0 + sl], start=True, stop=True),
                             reads=[('wi', d), 'xb'], writes=[('bank', 1)])
                        P.op('act', lambda e, i, d=d, s0=s0, sl=sl, k=k: e.activation(out=av[:, s0:s0 + sl], in_=C.banks[0][:, :sl], func=AF.Sigmoid,
                                                                                    bias=lv[:, k, 1 + d:2 + d]), reads=[('bank', 0), 'lv'], writes=['a'])
                        P.op('act', lambda e, i, d=d, s0=s0, sl=sl, k=k: e.activation(out=uv[:, s0:s0 + sl], in_=C.banks[1][:, :sl], func=AF.Sigmoid,
                                                                                    bias=lv[:, k, 3 + d:4 + d]), reads=[('bank', 1), 'lv'], writes=['u'])
                    P.op('dve', lambda e, i: e.tensor_tensor(out=uv[:, :], in0=uv[:, :], in1=xc_[:, :], op=ALU.mult), reads=['u', 'xc'], writes=['u'])
                    tgt = hf if d == 0 else hb_
                    P.op('act', lambda e, i, d=d, k=k, tgt=tgt: e.activation(out=tgt[:, :], in_=av[:, :], func=AF.Exp, scale=clam[:, k, 2 + d:3 + d]),
                         reads=['a', ('cl2', d)], writes=[('h', d)])
                    P.op('act', lambda e, i, d=d, k=k, tgt=tgt: e.activation(out=tgt[:, :], in_=tgt[:, :], func=AF.Sqrt, scale=-1.0, bias=one1[:, 0:1]),
                         reads=[('h', d), 'one1'], writes=[('h', d)])
                    P.op('dve', lambda e, i, tgt=tgt: e.tensor_tensor(out=uv[:, :], in0=uv[:, :], in1=tgt[:, :], op=ALU.mult), reads=['u', ('h', d)], writes=['u'])
                    P.op('act', lambda e, i, d=d, k=k: e.activation(out=av[:, :], in_=av[:, :], func=AF.Exp, scale=clam[:, k, d:d + 1]),
                         reads=['a', ('cl', d)], writes=['a'])
                    if d == 0:
                        P.op('dve', lambda e, i: e.tensor_tensor_scan(out=hf[:, SEQ:NTOK], data0=av[:, SEQ:NTOK], data1=uv[:, SEQ:NTOK], initial=0.0, op0=ALU.mult, op1=ALU.add),
                             reads=['a', 'u', ('h', 0)], writes=[('h', 0)])
                        P.op('dve', lambda e, i: e.tensor_tensor_scan(out=hf[:, 0:SEQ], data0=av[:, 0:SEQ], data1=uv[:, 0:SEQ], initial=hf[:, NTOK - 1:NTOK], op0=ALU.mult, op1=ALU.add),
                             reads=['a', 'u', ('h', 0)], writes=[('h', 0)])
                    else:
                        rev = lambda t, a0, n: bass.AP(t[:, :].tensor, t[:, a0 + n - 1:a0 + n].offset, [[t[:, :].ap[0][0], 128], [-1, n]])
                        P.op('dve', lambda e, i: e.tensor_tensor_scan(out=rev(hb_, SEQ, LCTX), data0=rev(av, SEQ, LCTX), data1=rev(uv, SEQ, LCTX), initial=0.0, op0=ALU.mult, op1=ALU.add),
                             reads=['a', 'u', ('h', 1)], writes=[('h', 1)])
                        P.op('dve', lambda e, i: e.tensor_tensor_scan(out=rev(hb_, 0, SEQ), data0=rev(av, 0, SEQ), data1=rev(uv, 0, SEQ), initial=hb_[:, SEQ:SEQ + 1], op0=ALU.mult, op1=ALU.add),
                             reads=['a', 'u', ('h', 1)], writes=[('h', 1)])
                P.op('dve', lambda e, i: e.tensor_tensor(out=hf[:, 0:SEQ], in0=hf[:, 0:SEQ], in1=hb_[:, 0:SEQ], op=ALU.add), reads=[('h', 0), ('h', 1)], writes=[('h', 0)])
                P.op('dve', lambda e, i: e.tensor_tensor(out=hb_[:, 0:NL], in0=hf[:, NL:SEQ], in1=hf[:, 0:NL], op=ALU.subtract), reads=[('h', 0)], writes=[('h', 1)])
                P.op('dve', lambda e, i, k=k: e.scalar_tensor_tensor(out=hb_[:, 0:NL], in0=hb_[:, 0:NL], scalar=lv[:, k, 7:8], in1=hf[:, 0:NL], op0=ALU.mult, op1=ALU.add),
                     reads=[('h', 0), ('h', 1), 'lv'], writes=[('h', 1)])
                P.op('dve', lambda e, i: e.tensor_tensor(out=oo[:, :], in0=hb_[:, 0:NL], in1=lyt[:, :], op=ALU.mult), reads=[('h', 1), 'ly'], writes=['oo'])
                P.dma('sp', CAT[2048 + k * 128:2048 + (k + 1) * 128, :], oo[:, :], reads=['oo'])

        stage_lin_out(C, CAT, D, OWNL_BLOCKS, [w_out], D, OT, "wo1" + tg)
        stage_resid_norm(C, R, xO, OT, XM, H2, blocks=[(i * 256, 256, 0) for i in range(8)], ident=True, HF=H2F)

        with stage(C):
            hx = P.sb("mhx", [128, KC, 512], F32)
            rw = Ring(P, "mrw", [128, 8, 128], F32, 3)
            L = P.sb("mL", [128, 8, 512], F32)
            T1 = P.sb("mT1", [128, 512], F32)
            T2 = P.sb("mT2", [128, 512], F32)
            M1 = P.sb("mM1", [128, 512], F32)
            M2 = P.sb("mM2", [128, 512], F32)
            E = P.sb("mE", [128, 8, 512], F32)
            hv = H2F.rearrange("(c p) t -> p c t", p=128)
            for tb_ in range(4):
                t0 = tb_ * 512
                for q in range(4):
                    P.dma('sp', hx[:, q * 8:(q + 1) * 8, :], hv[:, q * 8:(q + 1) * 8, t0:t0 + 512], writes=[('hx', q)])
                for kc in range(KC):
                    w_, wk = rw.next()
                    P.dma('act', w_[:, :, :], routerB[:, kc * 128:(kc + 1) * 128, :].rearrange("e p m -> p e m"), writes=[wk])
                    for ex in range(8):
                        P.op('pe', lambda e, i, ex=ex, kc=kc, w_=w_: e.matmul(C.banks[ex][:, :], lhsT=w_[:, ex, :], rhs=hx[:, kc, :], start=(kc == 0), stop=(kc == KC - 1)),
                             reads=[wk, ('hx', kc // 8)], writes=[('bank', ex)])
                for ex in range(8):
                    copy_op(C, C.alt(), L[:, ex, :], C.banks[ex][:, :], [('bank', ex)], [('L', ex)])
                allL = [('L', ex) for ex in range(8)]
                P.op('dve', lambda e, i: e.tensor_tensor(out=M1[:, :], in0=L[:, 0, :], in1=L[:, 1, :], op=ALU.max), reads=allL, writes=['M1'])
                for ex in range(2, 8):
                    P.op('dve', lambda e, i, ex=ex: e.tensor_tensor(out=M1[:, :], in0=M1[:, :], in1=L[:, ex, :], op=ALU.max), reads=allL + ['M1'], writes=['M1'])
                for ex in range(8):
                    P.op('dve', lambda e, i, ex=ex: e.tensor_tensor(out=T1[:, :], in0=L[:, ex, :], in1=M1[:, :], op=ALU.is_ge), reads=allL + ['M1'], writes=['T1'])
                    P.op('dve', lambda e, i, ex=ex: e.scalar_tensor_tensor(out=T1[:, :], in0=T1[:, :], scalar=-1e30, in1=L[:, ex, :], op0=ALU.mult, op1=ALU.add),
                         reads=['T1'] + allL, writes=['T1'])
                    if ex == 0:
                        P.op('dve', lambda e, i: e.tensor_copy(out=M2[:, :], in_=T1[:, :]), reads=['T1'], writes=['M2'])
                    else:
                        P.op('dve', lambda e, i: e.tensor_tensor(out=M2[:, :], in0=M2[:, :], in1=T1[:, :], op=ALU.max), reads=['T1', 'M2'], writes=['M2'])
                for ex in range(8):
                    P.op('dve', lambda e, i, ex=ex: e.tensor_tensor(out=T1[:, :], in0=L[:, ex, :], in1=M1[:, :], op=ALU.subtract), reads=allL + ['M1'], writes=['T1'])
                    P.op('act', lambda e, i: e.activation(out=T1[:, :], in_=T1[:, :], func=AF.Exp), reads=['T1'], writes=['T1'])
                    P.op('dve', lambda e, i, ex=ex: e.tensor_tensor(out=T2[:, :], in0=L[:, ex, :], in1=M2[:, :], op=ALU.is_ge), reads=allL + ['M2'], writes=['T2'])
                    P.op('dve', lambda e, i, ex=ex: e.tensor_tensor(out=E[:, ex, :], in0=T1[:, :], in1=T2[:, :], op=ALU.mult), reads=['T1', 'T2'], writes=[('E', ex)])
                allE = [('E', ex) for ex in range(8)]
                P.op('dve', lambda e, i: e.tensor_tensor(out=T1[:, :], in0=E[:, 0, :], in1=E[:, 1, :], op=ALU.add), reads=allE, writes=['T1'])
                for ex in range(2, 8):
                    P.op('dve', lambda e, i, ex=ex: e.tensor_tensor(out=T1[:, :], in0=T1[:, :], in1=E[:, ex, :], op=ALU.add), reads=allE + ['T1'], writes=['T1'])
                P.op('dve', lambda e, i: e.reciprocal(out=T1[:, :], in_=T1[:, :]), reads=['T1'], writes=['T1'])
                for ex in range(8):
                    P.op('dve', lambda e, i, ex=ex: e.tensor_tensor(out=E[:, ex, :], in0=E[:, ex, :], in1=T1[:, :], op=ALU.mult), reads=allE + ['T1'], writes=[('E', ex)])
                    P.dma('sp', COMB[ex, :, t0:t0 + 512], E[:, ex, :], reads=[('E', ex)])

        with stage(C):
            xs_b = P.sb("exs", [128, KC, 1024], BF16)
            wbs = [[P.sb(f"ewb{m}_{i}", [128, 256], BF16) for i in range(6)] for m in range(2)]
            cb_ = P.sb("ecb", [128, 1024], F32)
            tm = Ring(P, "etm", [128, 512], F32, 2)
            ob_ = Ring(P, "eob", [128, 512], BF16, 4)
            for (t0, tl) in OWNL_BLOCKS:
                for ex in range(8):
                    P.dma('act', cb_[:, :tl], COMB[ex, :, t0:t0 + tl], writes=['cb'])

                    def ev_e(j, si, c, t0_, s, sl, bl, ex=ex):
                        bg, bu = bl
                        t, tk = tm.next()
                        o, ok = ob_.next()
                        P.op('act', lambda e, i: e.activation(out=t[:, :sl], in_=C.banks[bg][:, :sl], func=AF.Silu), reads=[('bank', bg)], writes=[tk])
                        P.op('dve', lambda e, i: e.tensor_tensor(out=t[:, :sl], in0=t[:, :sl], in1=C.banks[bu][:, :sl], op=ALU.mult), reads=[tk, ('bank', bu)], writes=[tk])
                        P.op('dve', lambda e, i: e.tensor_tensor(out=o[:, :sl], in0=t[:, :sl], in1=cb_[:, s:s + sl], op=ALU.mult), reads=[tk, 'cb'], writes=[ok])
                        P.dma('sp', FH[ex * D + c:ex * D + c + 128, t0_ + s:t0_ + s + sl], o[:, :sl], reads=[ok])
                    lin(C, None, H2, D, [(t0, tl)], [mwg[ex], mwu[ex]], D, 'fm', ev_e, "e" + tg, ntile=2, bufs=(xs_b, wbs))

        with stage(C):
            xs_b = P.sb("dxs", [128, KC, 512], BF16)
            wbs = [[P.sb(f"dwb_{i}", [128, 512], BF16) for i in range(6)]]
            yacc = P.sb("dy", [128, KC, 512], F32)
            yv = YT.rearrange("(c p) t -> p c t", p=128)
            for tb_ in range(4):
                t0 = tb_ * 512
                for ex in range(8):
                    def ev_d(j, si, c, t0_, s, sl, bl, ex=ex):
                        ct = c // 128
                        b = bl[0]
                        if ex == 0:
                            copy_op(C, C.alt(), yacc[:, ct, :sl], C.banks[b][:, :sl], [('bank', b)], [('y', ct)])
                        else:
                            P.op('dve', lambda e, i: e.tensor_tensor(out=yacc[:, ct, :sl], in0=yacc[:, ct, :sl], in1=C.banks[b][:, :sl], op=ALU.add),
                                 reads=[('bank', b), ('y', ct)], writes=[('y', ct)])
                    lin(C, None, FH[ex * D:(ex + 1) * D, :], D, [(t0, 512)], [mwd[ex]], D, 'fm', ev_d, "d" + tg, bufs=(xs_b, wbs), tb=512)
                for q in range(4):
                    P.dma('sp', yv[:, q * 8:(q + 1) * 8, t0:t0 + 512], yacc[:, q * 8:(q + 1) * 8, :], reads=[('y', ct) for ct in range(q * 8, q * 8 + 8)])

        stage_final(C, R, XM, YT, out_l, None, blocks=[(i * 256, 256, 0) for i in range(8)])
        C.P.flush()
        C.P.stack = old_stack


def l1_constants(s):
    inv = (10000.0 ** (-np.arange(16, dtype=np.float32) / 16)).astype(np.float32)
    p = np.arange(128) % 64
    qd = p // 16
    f = p % 16
    sign = np.where(qd % 2 == 0, -1.0, 1.0).astype(np.float32)[:, None]

    def tab(tact):
        row = (tact // 64).astype(np.float32)
        col = (tact % 64).astype(np.float32)
        pos = np.where((qd < 2)[:, None], row[None, :], col[None, :]).astype(np.float32)
        ang = (pos * inv[f][:, None]).astype(np.float32)
        return np.stack([np.cos(ang), sign * np.sin(ang)]).astype(np.float32)
    return dict(ropeK=tab(np.arange(SEQ)), ropeQ=tab(2048 * s + np.arange(NL)))


def l1_shared(inp):
    wio = inp['w_in_odd'][0]
    w_in = np.concatenate([wio[:, 0:2048], wio[:, 2048:2112], wio[:, 2048:2112], wio[:, 2112:6208]], axis=1)
    wq = inp['mla_w_uq'][0].reshape(1536, 16, 192)
    wuq = np.concatenate([wq[:, :, :128].reshape(1536, 2048), wq[:, :, 128:].reshape(1536, 1024)], axis=1)
    wkv = inp['mla_w_ukv'][0].reshape(512, 16, 256)
    wukv = np.concatenate([wkv[:, :, :128].reshape(512, 2048), wkv[:, :, 128:].reshape(512, 2048)], axis=1)
    cw = np.ascontiguousarray(np.asarray(inp['lru_conv_w'][0], np.float32).reshape(4, 16, 128).transpose(2, 1, 0))
    rB = np.ascontiguousarray(np.broadcast_to(np.asarray(inp['router_w'][0], np.float32).T[:, :, None], (8, D, 128)))
    return {
        "w_in": np.ascontiguousarray(w_in), "gq": np.ascontiguousarray(np.asarray(inp['mla_q_norm'][0], np.float32).reshape(12, 128).T),
        "wuq": np.ascontiguousarray(wuq), "gkv": np.ascontiguousarray(np.asarray(inp['mla_kv_norm'][0], np.float32).reshape(4, 128).T),
        "wukv": np.ascontiguousarray(wukv), "cw": cw,
        "wa": inp['lru_w_a'][0], "wi": inp['lru_w_i'][0], "w_out": inp['w_out_odd'][0], "routerB": rB,
        "mwg": inp['moe_w_gate'][0], "mwu": inp['moe_w_up'][0], "mwd": inp['moe_w_down'][0],
        "ropeK": l1_constants(0)['ropeK'],
    }


def l1_inputs(inp, mod, xl0, xc0, b, shared):
    m = dict(shared)
    xt = np.concatenate([xl0[b], xc0[b]], axis=0)
    vec = np.zeros((128, KC, 16), np.float32)
    for j in range(6):
        vec[:, :, j] = _pvec(mod[1, b, j])
        vec[:, :, 6 + j] = _pvec(mod[1, 4, j])
    for j in range(4):
        vec[:, :, 12 + j] = _pvec(inp['norm_g'][1, j])
    m["xT"] = np.ascontiguousarray(xt.T)
    m["vec"] = vec
    ch = lambda v: np.ascontiguousarray(np.asarray(v, np.float32).reshape(16, 128).T)
    for s in range(NUNITS):
        lvec = np.zeros((128, 16, 8), np.float32)
        lvec[:, :, 0] = ch(inp['lru_conv_b'][0])
        for d in range(2):
            lvec[:, :, 1 + d] = ch(inp['lru_b_a'][0, d])
            lvec[:, :, 3 + d] = ch(inp['lru_b_i'][0, d])
            lvec[:, :, 5 + d] = ch(inp['lru_lambda'][0, d])
        lvec[:, :, 7] = float(s)
        m[f"lvec_{s}"] = lvec
        m[f"xO_{s}"] = np.ascontiguousarray(xl0[b, 2048 * s:2048 * (s + 1)].T)
        m[f"ropeQ_{s}"] = l1_constants(s)['ropeQ']
    return m


def run_layer1(inp, mod, xl0, xc0):
    assert NUNITS == 2
    shared = l1_shared(inp)
    in_maps = [l1_inputs(inp, mod, xl0, xc0, b, shared) for b in range(NCORES)]
    res = run_bass_kernel_spmd(build_layer1(), in_maps, core_ids=list(range(NCORES)))
    B = xl0.shape[0]
    out = np.empty((B, SEQ, D), np.float32)
    for b in range(NCORES):
        for s in range(NUNITS):
            out[b, 2048 * s:2048 * (s + 1)] = res.results[b][f"out_l_{s}"].T
    return out


def build_mod(ncols):
    NT = ncols // 128
    nc = _new_nc()
    cT = nc.dram_tensor("cT", [128, KC, 5], F32, kind="ExternalInput").ap()
    Wm = nc.dram_tensor("Wm", [D, ncols], F32, kind="ExternalInput").ap()
    bm = nc.dram_tensor("bm", [128, NT], F32, kind="ExternalInput").ap()
    mo = nc.dram_tensor("mo", [128, NT, 5], F32, kind="ExternalOutput").ap()
    with ExitStack() as st:
        P = Prog(nc, st)
        c_sb = P.sb("c_sb", [128, KC, 5], F32)
        s_sb = P.sb("s_sb", [128, KC, 5], F32)
        b_sb = P.sb("b_sb", [128, NT], F32)
        o_sb = P.sb("o_sb", [128, NT, 5], F32)
        NW = 3
        wt = [P.sb(f"wt{i}", [128, ncols], F32) for i in range(NW)]
        acc = P.ps("acc", [128, 512])
        P.dma('sp', c_sb[:, :, :], cT[:, :, :], writes=['c'])
        P.dma('sp', b_sb[:, :], bm[:, :], writes=['b'])
        P.op('act', lambda e, i: e.activation(out=s_sb[:, :, :], in_=c_sb[:, :, :], func=AF.Silu), reads=['c'], writes=['s'])
        for kc in range(KC):
            w = kc % NW
            P.dma('sp' if kc % 2 == 0 else 'pool', wt[w][:, :], Wm[kc * 128:(kc + 1) * 128, :], writes=[('w', w)])
            for nt in range(NT):
                P.op('pe', lambda e, i, w=w, nt=nt, kc=kc: e.matmul(
                    acc[:, nt * 5:(nt + 1) * 5], lhsT=wt[w][:, nt * 128:(nt + 1) * 128], rhs=s_sb[:, kc, :],
                    start=(kc == 0 and nt == 0), stop=(kc == KC - 1 and nt == NT - 1), skip_group_check=True),
                    reads=[('w', w), 's'], writes=['acc'])
        accv = acc[:, 0:NT * 5].rearrange("p (n r) -> p n r", r=5)
        for r in range(5):
            P.op('dve', lambda e, i, r=r: e.tensor_tensor(out=o_sb[:, :, r], in0=accv[:, :, r], in1=b_sb[:, :], op=ALU.add),
                 reads=['acc', 'b'], writes=[('o', r)])
        P.dma('sp', mo[:, :, :], o_sb[:, :, :], reads=[('o', r) for r in range(5)])
        P.emit()
    return nc


def run_mod(inp, NCORES=8):
    cc = np.concatenate([np.asarray(inp['c'], np.float32), np.asarray(inp['c_ctx'], np.float32)[None]], axis=0)
    cT = np.ascontiguousarray(cc.reshape(5, KC, 128).transpose(2, 1, 0))
    w_mod = inp['w_mod']
    b_mod = np.asarray(inp['b_mod'], np.float32)
    ncols = 2 * 6 * D // NCORES
    NT = ncols // 128
    in_maps = []
    for i in range(NCORES):
        layer, part = divmod(i, NCORES // 2)
        c0 = part * ncols
        in_maps.append({"cT": cT, "Wm": np.ascontiguousarray(w_mod[layer][:, c0:c0 + ncols], dtype=np.float32),
                        "bm": np.ascontiguousarray(b_mod[layer, c0:c0 + ncols].reshape(NT, 128).T)})
    res = run_bass_kernel_spmd(build_mod(ncols), in_maps, core_ids=list(range(NCORES)))
    mod = np.zeros((2, 5, 6 * D), np.float32)
    for i in range(NCORES):
        layer, part = divmod(i, NCORES // 2)
        mod[layer][:, part * ncols:(part + 1) * ncols] = res.results[i]["mo"].transpose(2, 1, 0).reshape(5, ncols)
    return mod.reshape(2, 5, 6, D)


def run_layer0(inp, mod):
    assert NUNITS == 2
    consts = [l0_constants(s) for s in range(2)]
    shared = {"w_in": inp['w_in_even'][0], "w_out": inp['w_out_even'][0],
              "wg": inp['ffn_w_gate'][0], "wu": inp['ffn_w_up'][0], "wd": inp['ffn_w_down'][0]}
    in_maps = []
    for b in range(NCORES):
        m = dict(shared)
        for s in range(NUNITS):
            for kk, v in l0_inputs(inp, mod, b, s, consts[s]).items():
                m[f"{kk}_{s}"] = v
        in_maps.append(m)
    res = run_bass_kernel_spmd(build_layer0(), in_maps, core_ids=list(range(NCORES)))
    B = inp['x'].shape[0]
    xl = np.empty((B, SEQ, D), np.float32)
    xc = np.empty((B, LCTX, D), np.float32)
    for b in range(NCORES):
        for s in range(NUNITS):
            xl[b, 2048 * s:2048 * (s + 1)] = res.results[b][f"out_l_{s}"].T
            xc[b, 128 * s:128 * (s + 1)] = res.results[b][f"out_c_{s}"].T
    return xl, xc


def kernel(**inp):
    inp = {k: np.asarray(v) for k, v in inp.items()}
    mod = run_mod(inp)
    xl, xc = run_layer0(inp, mod)
    out = run_layer1(inp, mod, xl, xc)
    return out
```
